# Optimizing a Trainium2 kernel written in Bass

```python
import jax, jax.numpy as jnp
from jax import lax
import numpy as np

D_MODEL = 1024
BATCH = 16
SEQ = 2048
DEPTH = 1

CTX_LEN = 256
GRID_W = 64
D_MIX = 2 * D_MODEL
D_SSD = D_MIX // 2
D_SC = D_MIX - D_SSD
SSD_HEAD_DIM = 64
SSD_HEADS = D_SSD // SSD_HEAD_DIM
SSD_GROUPS = 2
SSD_STATE = 128
SSD_CONV = 3
SSD_CHUNK = 128
SC_CONV = 3
SC_GROUPS = D_SC // 64
N_EXPERTS = 32
TOP_K = 4
D_FF = D_MODEL
SWIGLU_LIMIT = 7.0
SWIGLU_ALPHA = 1.702
MOE_BLOCK = 256
NORM_EPS = 1e-6
N_MOD = 6

Z0 = 0
X0 = Z0 + D_SSD
B0 = X0 + D_SSD
C0 = B0 + SSD_GROUPS * SSD_STATE
DT0 = C0 + SSD_GROUPS * SSD_STATE
SC0 = DT0 + 2 * SSD_HEADS
IN_W = SC0 + 3 * D_SC
XBC_W = DT0 - X0

kernel_name = "hybrid_ssd_shortconv_moe_dit_layer"


def rmsnorm(x, g):
    xf = x.astype(jnp.float32)
    y = xf * lax.rsqrt(jnp.mean(xf * xf, axis=-1, keepdims=True) + NORM_EPS)
    return (y * g.astype(jnp.float32)).astype(x.dtype)


def group_rmsnorm(x, g, groups):
    shp = x.shape
    xf = x.astype(jnp.float32).reshape(*shp[:-1], groups, shp[-1] // groups)
    y = xf * lax.rsqrt(jnp.mean(xf * xf, axis=-1, keepdims=True) + NORM_EPS)
    return (y.reshape(shp) * g.astype(jnp.float32)).astype(x.dtype)


def modulate(h, shift, scale):
    return h * (1 + scale) + shift


def flip(t):
    return t[:, ::-1]


def conv_seq(u, w):
    k_w = w.shape[0]
    pad = k_w // 2
    l = u.shape[1]
    up = jnp.pad(u, ((0, 0), (pad, pad), (0, 0)))
    return sum(up[:, k:k + l] * w[k] for k in range(k_w))


def conv_grid_cols(u, w):
    b, l, ch = u.shape
    rows = l // GRID_W
    k_w = w.shape[0]
    pad = k_w // 2
    ug = jnp.pad(u.reshape(b, rows, GRID_W, ch), ((0, 0), (pad, pad), (0, 0), (0, 0)))
    out = sum(ug[:, k:k + rows] * w[k] for k in range(k_w))
    return out.reshape(b, l, ch)


def ssd_decays(dt_raw, dt_bias, a_log):
    b, l, _ = dt_raw.shape
    dt = jax.nn.softplus(dt_raw.astype(jnp.float32).reshape(b, l, 2, SSD_HEADS) + dt_bias.astype(jnp.float32))
    a = -jnp.exp(a_log.astype(jnp.float32))
    return dt[:, :, 0], dt[:, :, 1], a[0], a[1]


def ssd_chunked(xs, dt, a, bm, cm, h0):
    b, l, h, p = xs.shape
    g, n = bm.shape[2], bm.shape[3]
    r = h // g
    nc = l // SSD_CHUNK
    q = SSD_CHUNK
    xdt = (xs.astype(jnp.float32) * dt[..., None]).reshape(b, nc, q, g, r, p)
    acs = jnp.cumsum((dt * a).reshape(b, nc, q, g, r), axis=2)
    bc = bm.astype(jnp.float32).reshape(b, nc, q, g, n)
    cc = cm.astype(jnp.float32).reshape(b, nc, q, g, n)
    seg = acs[:, :, :, None] - acs[:, :, None, :]
    lower = jnp.tril(jnp.ones((q, q), dtype=bool))[:, :, None, None]
    decay_ij = jnp.exp(jnp.where(lower, seg, -jnp.inf))
    cb = jnp.einsum('bcign,bcjgn->bcijg', cc, bc)
    y_diag = jnp.einsum('bcijg,bcijgr,bcjgrp->bcigrp', cb, decay_ij, xdt)
    decay_to_end = jnp.exp(acs[:, :, -1:] - acs)
    states = jnp.einsum('bcqgn,bcqgr,bcqgrp->bcgrpn', bc, decay_to_end, xdt)
    chunk_decay = jnp.exp(acs[:, :, -1])

    def step(hc, inp):
        dec, st = inp
        return hc * dec[..., None, None] + st, hc

    h_final, starts = lax.scan(step, h0.astype(jnp.float32).reshape(b, g, r, p, n),
                               (jnp.moveaxis(chunk_decay, 1, 0), jnp.moveaxis(states, 1, 0)))
    starts = jnp.moveaxis(starts, 0, 1)
    y_off = jnp.einsum('bcign,bcgrpn,bcigr->bcigrp', cc, starts, jnp.exp(acs))
    y = (y_diag + y_off).reshape(b, l, h, p).astype(xs.dtype)
    return y, h_final.reshape(b, h, p, n)


def ssd_final_state(xs, dt, a, bm):
    b, l, h, p = xs.shape
    g, n = bm.shape[2], bm.shape[3]
    r = h // g
    acs = jnp.cumsum(dt * a, axis=1)
    w = (jnp.exp(acs[:, -1:] - acs) * dt).reshape(b, l, g, r)
    st = jnp.einsum('blgn,blgr,blgrp->bgrpn', bm.astype(jnp.float32), w,
                    xs.astype(jnp.float32).reshape(b, l, g, r, p))
    return st.reshape(b, h, p, n)


def ssd_bidir(xs, bm, cm, dt_f, dt_b, a_f, a_b, d_skip, h0_f, h0_b):
    y_f, hf = ssd_chunked(xs, dt_f, a_f, bm, cm, h0_f)
    y_b, hb = ssd_chunked(flip(xs), flip(dt_b), a_b, flip(bm), flip(cm), h0_b)
    y = y_f + flip(y_b) + d_skip[:, None].astype(xs.dtype) * xs
    return y, hf, hb


def token_mixers(h, w_in, ssd_conv_w, ssd_conv_b, dt_bias, a_log, d_skip, ssd_norm_g, sc_conv_w, w_out,
                 sc_conv, h0_f, h0_b):
    b, l, _ = h.shape
    proj = h @ w_in
    z = proj[..., Z0:X0]
    xbc = jax.nn.silu(conv_seq(proj[..., X0:DT0], ssd_conv_w) + ssd_conv_b)
    xs = xbc[..., :B0 - X0].reshape(b, l, SSD_HEADS, SSD_HEAD_DIM)
    bm = xbc[..., B0 - X0:C0 - X0].reshape(b, l, SSD_GROUPS, SSD_STATE)
    cm = xbc[..., C0 - X0:].reshape(b, l, SSD_GROUPS, SSD_STATE)
    dt_f, dt_b, a_f, a_b = ssd_decays(proj[..., DT0:SC0], dt_bias, a_log)
    y, hf, hb = ssd_bidir(xs, bm, cm, dt_f, dt_b, a_f, a_b, d_skip, h0_f, h0_b)
    y_ssd = group_rmsnorm(y.reshape(b, l, D_SSD) * jax.nn.silu(z), ssd_norm_g, SSD_GROUPS)
    sc_b = proj[..., SC0:SC0 + D_SC]
    sc_c = proj[..., SC0 + D_SC:SC0 + 2 * D_SC]
    sc_u = proj[..., SC0 + 2 * D_SC:]
    y_sc = sc_b * sc_conv(sc_c * sc_u, sc_conv_w)
    out = jnp.concatenate([y_ssd, y_sc], axis=-1) @ w_out
    return out, hf, hb


def context_ssd_states(hc, w_in, ssd_conv_w, ssd_conv_b, dt_bias, a_log):
    b, l, _ = hc.shape
    xb = jax.nn.silu(conv_seq(hc @ w_in[:, X0:C0], ssd_conv_w[:, :C0 - X0]) + ssd_conv_b[:C0 - X0])
    xs = xb[..., :B0 - X0].reshape(b, l, SSD_HEADS, SSD_HEAD_DIM)
    bm = xb[..., B0 - X0:].reshape(b, l, SSD_GROUPS, SSD_STATE)
    dt_f, dt_b, a_f, a_b = ssd_decays(hc @ w_in[:, DT0:SC0], dt_bias, a_log)
    h_f = ssd_final_state(xs, dt_f, a_f, bm)
    h_b = ssd_final_state(flip(xs), flip(dt_b), a_b, flip(bm))
    return h_f, h_b


def moe(h, w_router, b_router, w_gate_up, b_gate_up, w_down, b_down):
    shp = h.shape
    d = shp[-1]
    t = h.reshape(-1, d)
    n_tok = t.shape[0]
    n_assign = n_tok * TOP_K
    logits = (t @ w_router + b_router).astype(jnp.float32)
    top_logits, top_idx = lax.top_k(logits, TOP_K)
    gates = jax.nn.softmax(top_logits, axis=-1).astype(h.dtype)
    expert = top_idx.reshape(-1).astype(jnp.int32)
    order = jnp.argsort(expert)
    expert_sorted = expert[order]
    counts = jnp.bincount(expert, length=N_EXPERTS).astype(jnp.int32)
    padded = (counts + MOE_BLOCK - 1) // MOE_BLOCK * MOE_BLOCK
    group_start = jnp.cumsum(counts) - counts
    pad_end = jnp.cumsum(padded)
    pad_start = pad_end - padded
    dest_sorted = (pad_start[expert_sorted] + jnp.arange(n_assign, dtype=jnp.int32)
                   - group_start[expert_sorted]).astype(jnp.int32)
    dest = jnp.zeros((n_assign,), jnp.int32).at[order].set(dest_sorted)
    n_blocks = -(-n_assign // MOE_BLOCK) + N_EXPERTS
    row_token = jnp.full((n_blocks * MOE_BLOCK,), n_tok, jnp.int32).at[dest].set(
        jnp.arange(n_assign, dtype=jnp.int32) // TOP_K)
    t_ext = jnp.concatenate([t, jnp.zeros((1, d), t.dtype)], axis=0)
    blocks = t_ext[row_token].reshape(n_blocks, MOE_BLOCK, d)
    block_expert = jnp.minimum(
        jnp.searchsorted(pad_end, jnp.arange(n_blocks, dtype=jnp.int32) * MOE_BLOCK, side='right'),
        N_EXPERTS - 1)

    def expert_block(args):
        xb, e = args
        gu = xb @ w_gate_up[e] + b_gate_up[e]
        glu = jnp.minimum(gu[:, :D_FF], SWIGLU_LIMIT)
        lin = jnp.clip(gu[:, D_FF:], -SWIGLU_LIMIT, SWIGLU_LIMIT)
        act = glu * jax.nn.sigmoid(SWIGLU_ALPHA * glu) * (lin + 1)
        return act @ w_down[e] + b_down[e]

    y_rows = lax.map(expert_block, (blocks, block_expert)).reshape(-1, d)
    y = y_rows[dest].reshape(n_tok, TOP_K, d)
    return jnp.einsum('tk,tkd->td', gates, y).reshape(shp)


def setup_inputs(seed: int = 0) -> dict:
    key = jax.random.key(seed)
    ks = jax.random.split(key, 26)

    def nrm(k, shape, scale):
        return jax.random.normal(k, shape, jnp.float32) * scale

    dt0 = jnp.exp(jax.random.uniform(ks[10], (DEPTH, 2, SSD_HEADS), jnp.float32,
                                     minval=float(np.log(1e-3)), maxval=float(np.log(1e-1))))
    dt_bias = dt0 + jnp.log(-jnp.expm1(-dt0))
    a_log = jnp.log(jax.random.uniform(ks[11], (DEPTH, 2, SSD_HEADS), jnp.float32, minval=1.0, maxval=16.0))
    return {
        "x": nrm(ks[0], (BATCH, SEQ, D_MODEL), 1.0),
        "c": nrm(ks[1], (BATCH, D_MODEL), 1.0),
        "ctx": nrm(ks[2], (BATCH, CTX_LEN, D_MODEL), 1.0),
        "c_ctx": nrm(ks[3], (D_MODEL,), 1.0),
        "w_mod": nrm(ks[4], (DEPTH, D_MODEL, N_MOD * D_MODEL), 0.5 * D_MODEL ** -0.5),
        "b_mod": nrm(ks[5], (DEPTH, N_MOD * D_MODEL), 0.02),
        "norm1_g": 1.0 + nrm(ks[6], (DEPTH, D_MODEL), 0.02),
        "w_in": nrm(ks[7], (DEPTH, D_MODEL, IN_W), D_MODEL ** -0.5),
        "ssd_conv_w": nrm(ks[8], (DEPTH, SSD_CONV, XBC_W), SSD_CONV ** -0.5),
        "ssd_conv_b": nrm(ks[9], (DEPTH, XBC_W), 0.02),
        "ssd_dt_bias": dt_bias,
        "ssd_a_log": a_log,
        "ssd_d": 1.0 + nrm(ks[12], (DEPTH, SSD_HEADS), 0.1),
        "ssd_norm_g": 1.0 + nrm(ks[13], (DEPTH, D_SSD), 0.02),
        "sc_conv_w": nrm(ks[14], (DEPTH, SC_CONV, D_SC), SC_CONV ** -0.5),
        "w_out": nrm(ks[15], (DEPTH, D_MIX, D_MODEL), D_MIX ** -0.5),
        "norm2_g": 1.0 + nrm(ks[16], (DEPTH, D_MODEL), 0.02),
        "w_router": nrm(ks[17], (DEPTH, D_MODEL, N_EXPERTS), D_MODEL ** -0.5),
        "b_router": nrm(ks[18], (DEPTH, N_EXPERTS), 0.01),
        "w_gate_up": nrm(ks[19], (DEPTH, N_EXPERTS, D_MODEL, 2 * D_FF), D_MODEL ** -0.5),
        "b_gate_up": nrm(ks[20], (DEPTH, N_EXPERTS, 2 * D_FF), 0.02),
        "w_down": nrm(ks[21], (DEPTH, N_EXPERTS, D_FF, D_MODEL), D_FF ** -0.5),
        "b_down": nrm(ks[22], (DEPTH, N_EXPERTS, D_MODEL), 0.02),
        "final_g": 1.0 + nrm(ks[23], (D_MODEL,), 0.02),
    }


def reference(x, c, ctx, c_ctx, w_mod, b_mod, norm1_g, w_in, ssd_conv_w, ssd_conv_b, ssd_dt_bias, ssd_a_log,
              ssd_d, ssd_norm_g, sc_conv_w, w_out, norm2_g, w_router, b_router, w_gate_up, b_gate_up, w_down,
              b_down, final_g):
    b = x.shape[0]
    ctx_s = ctx
    for i in range(DEPTH):
        mod_x = (jax.nn.silu(c) @ w_mod[i] + b_mod[i]).reshape(b, N_MOD, 1, D_MODEL)
        mod_c = (jax.nn.silu(c_ctx) @ w_mod[i] + b_mod[i]).reshape(N_MOD, 1, 1, D_MODEL)
        mix_w = (w_in[i], ssd_conv_w[i], ssd_conv_b[i], ssd_dt_bias[i], ssd_a_log[i], ssd_d[i],
                 ssd_norm_g[i], sc_conv_w[i], w_out[i])
        moe_w = (w_router[i], b_router[i], w_gate_up[i], b_gate_up[i], w_down[i], b_down[i])
        hc = modulate(rmsnorm(ctx_s, norm1_g[i]), mod_c[0], mod_c[1])
        if i == DEPTH - 1:
            h0_f, h0_b = context_ssd_states(hc, w_in[i], ssd_conv_w[i], ssd_conv_b[i],
                                            ssd_dt_bias[i], ssd_a_log[i])
        else:
            zero = jnp.zeros((ctx_s.shape[0], SSD_HEADS, SSD_HEAD_DIM, SSD_STATE), jnp.float32)
            oc, h0_f, h0_b = token_mixers(hc, *mix_w, conv_seq, zero, zero)
            ctx_s = ctx_s + mod_c[2] * oc
            ctx_s = ctx_s + mod_c[5] * moe(modulate(rmsnorm(ctx_s, norm2_g[i]), mod_c[3], mod_c[4]), *moe_w)
        hx = modulate(rmsnorm(x, norm1_g[i]), mod_x[:, 0], mod_x[:, 1])
        ox, _, _ = token_mixers(hx, *mix_w, conv_grid_cols, h0_f, h0_b)
        x = x + mod_x[:, 2] * ox
        x = x + mod_x[:, 5] * moe(modulate(rmsnorm(x, norm2_g[i]), mod_x[:, 3], mod_x[:, 4]), *moe_w)
    return rmsnorm(x, final_g)
```

```python
import numpy as np
from contextlib import ExitStack
import concourse.bass as bass
import concourse.mybir as mybir
from concourse.bass_utils import run_bass_kernel_spmd

F32 = mybir.dt.float32
BF16 = mybir.dt.bfloat16
I32 = mybir.dt.int32
ALU = mybir.AluOpType
AF = mybir.ActivationFunctionType
AX = mybir.AxisListType

ENGS = ("pe", "dve", "act", "pool", "sp")

D = 1024
SEQ = 2048
CTX = 256
NB = 2
NT = SEQ // 128
IN_W = 5664
X0, B0, C0, DT0, SC0 = 1024, 2048, 2304, 2560, 2592
NE = 32
EPS = 1e-6
MBS = 384
MNBLK = -(-(NB * SEQ * 4 + NE * (MBS - 1)) // MBS)
HP = 16


class Op:
    __slots__ = ("eng", "fn", "deps", "needs_inc", "val", "is_dma", "dsem", "waits")

    def __init__(self, eng, fn, is_dma=False, dsem=None):
        self.eng = eng
        self.fn = fn
        self.deps = []
        self.needs_inc = False
        self.val = None
        self.is_dma = is_dma
        self.dsem = dsem
        self.waits = None


class Prog:
    def __init__(self, nc):
        self.nc = nc
        self.ops = {e: [] for e in ENGS}
        self.res = {}
        self.dma_counts = {}
        self.last_dma = {}
        self.out_dma_ops = []
        self.dmap = {}
        self.free_phys = []
        self.nphys = 0
        self.bg = set()
        self.bgkeys = {}
        self.free_by_cls = {}
        self.phys_cls = {}

    def _add(self, op, reads, writes):
        deps = []
        res = self.res
        for k in reads:
            st = res.get(k)
            if st is None:
                st = res[k] = [None, {}]
            if st[0] is not None:
                deps.append(st[0])
        for k in writes:
            st = res.get(k)
            if st is None:
                st = res[k] = [None, {}]
            if st[0] is not None:
                deps.append(st[0])
            deps.extend(st[1].values())
        rk = ("d", op.dsem) if op.is_dma else op.eng
        for k in reads:
            res[k][1][rk] = op
        for k in writes:
            st = res[k]
            st[0] = op
            st[1] = {}
        seen = set()
        for d in deps:
            if d is op or id(d) in seen:
                continue
            seen.add(id(d))
            if (not d.is_dma) and (not op.is_dma) and d.eng == op.eng and op.eng == "pe":
                continue
            op.deps.append(d)
            d.needs_inc = True
        self.ops[op.eng].append(op)
        return op

    def op(self, eng, fn, reads=(), writes=()):
        return self._add(Op(eng, fn), reads, writes)

    def dma(self, queue, fn, dsem, reads=(), writes=(), is_output=False, background=False):
        if background and dsem not in self.dmap:
            self.dmap[dsem] = self.nphys
            self.phys_cls[self.nphys] = "bg"
            self.bg.add(self.nphys)
            self.bgkeys[dsem] = self.nphys
            self.nphys += 1
        if dsem not in self.dmap:
            cls = "sw" if queue == "pool" else "hw"
            fl = self.free_by_cls.setdefault(cls, [])
            if fl:
                self.dmap[dsem] = fl.pop()
            else:
                self.dmap[dsem] = self.nphys
                self.phys_cls[self.nphys] = cls
                self.nphys += 1
        dsem = self.dmap[dsem]
        op = Op(queue, fn, is_dma=True, dsem=dsem)
        self.dma_counts[dsem] = self.dma_counts.get(dsem, 0) + 1
        op.val = 16 * self.dma_counts[dsem]
        op.needs_inc = True
        self._add(op, reads, writes)
        self.last_dma[dsem] = op
        if is_output:
            self.out_dma_ops.append(op)
        return op

    def barrier(self):
        lasts = []
        for e in ENGS:
            for op in reversed(self.ops[e]):
                if op.fn is not None and not op.is_dma:
                    lasts.append(op)
                    break
        dl = [op_ for k_, op_ in self.last_dma.items() if k_ not in self.bg]
        for e in ENGS:
            b = Op(e, None)
            b.deps = [d for d in lasts if not (d.eng == e and e == "pe")] + dl
            for d in b.deps:
                d.needs_inc = True
            self.ops[e].append(b)
        for v_ in self.dmap.values():
            if v_ not in self.bg:
                self.free_by_cls.setdefault(self.phys_cls[v_], []).append(v_)
        self.dmap = dict(self.bgkeys)

    def finalize_and_emit(self, stack):
        nc = self.nc
        fin = Op("sp", None)
        fin.deps = list(self.out_dma_ops)
        self.ops["sp"].append(fin)
        for e in ENGS:
            c = 0
            for op in self.ops[e]:
                if op.is_dma:
                    continue
                if op.needs_inc:
                    c += 1
                    op.val = c
        esem = {e: stack.enter_context(nc.semaphore("s_" + e)) for e in ENGS}
        dsems = {k: stack.enter_context(nc.semaphore("d_" + str(k))) for k in self.dma_counts}
        nwaits = 0
        for e in ENGS:
            seen = {}
            for op in self.ops[e]:
                w = {}
                for d in op.deps:
                    s = ("d", d.dsem) if d.is_dma else ("e", d.eng)
                    if seen.get(s, 0) >= d.val:
                        continue
                    if w.get(s, 0) < d.val:
                        w[s] = d.val
                for s, v in w.items():
                    seen[s] = v
                op.waits = [((dsems[s[1]] if s[0] == "d" else esem[s[1]]), v) for s, v in w.items()]
                nwaits += len(op.waits)
        self.stats = {e: len(self.ops[e]) for e in ENGS}
        self.stats["waits"] = nwaits
        self.stats["dsems"] = len(dsems)
        block = stack.enter_context(nc.Block())
        ops = self.ops

        def replay(engname, eng):
            for op in ops[engname]:
                for (s, v) in op.waits:
                    eng.wait_ge(s, v)
                if op.fn is None:
                    continue
                ins = op.fn(eng)
                if op.is_dma:
                    ins.then_inc(dsems[op.dsem], 16)
                elif op.needs_inc:
                    ins.then_inc(esem[engname], 1)

        @block.tensor
        def _(eng):
            replay("pe", eng)

        @block.vector
        def _(eng):
            replay("dve", eng)

        @block.scalar
        def _(eng):
            replay("act", eng)

        @block.gpsimd
        def _(eng):
            replay("pool", eng)

        @block.sync
        def _(eng):
            replay("sp", eng)


class _Stop(Exception):
    pass


class Arena:
    def __init__(self, ap, n):
        self.ap, self.n, self.off = ap, n, 0

    def f32(self, n):
        assert self.off + n <= self.n, ("arena overflow", self.off, n, self.n)
        v = self.ap[:, self.off:self.off + n]
        self.off += n
        return v

    def bf16(self, n):
        m = (n + 1) // 2
        assert self.off + m <= self.n, ("arena overflow", self.off, m, self.n)
        v = self.ap[:, self.off:self.off + m].bitcast(BF16)
        self.off += m
        return v

    def mark(self):
        return self.off

    def reset(self, m):
        self.off = m


def build(debug=False, stop_after=None):
    nc = bass.Bass("TRN2", target_bir_lowering=False)

    def din(name, shape):
        return nc.dram_tensor(name, shape, F32, kind="ExternalInput").ap()

    scr_kind = "ExternalOutput" if debug else "Internal"

    def dscr(name, shape, dt):
        return nc.dram_tensor(name, shape, dt, kind=scr_kind).ap()

    x_d = din("x", [NB, SEQ, D])
    ctx_d = din("ctx", [NB, CTX, D])
    cT_d = din("cT", [128, 8, 3])
    w_mod_d = din("w_mod", [D, 6 * D])
    b_modT_d = din("b_modT", [128, 48])
    b_mod_row_d = din("b_mod_row", [1, 6 * D])
    n1gT_d = din("n1gT", [128, 8])
    n2gT_d = din("n2gT", [128, 8])
    w_in_d = din("w_in", [D, IN_W])
    convw_d = din("convw", [3, 1536])
    convb_row_d = din("convb_row", [1, 1536])
    convbT_d = din("convbT", [128, 12])
    dtb_row_d = din("dtb_row", [1, 32])
    alog_row_d = din("alog_row", [1, 32])
    d_row_d = din("d_row", [1, 16])
    gssd_row_d = din("gssd_row", [1, D])
    scwT_d = din("scwT", [128, 8, 3])
    w_out_d = din("w_out", [2 * D, D])
    w_router_d = din("w_router", [D, NE])
    br_row_d = din("br_row", [1, NE])
    w_gu_d = din("w_gu", [NE, D, 2 * D])
    bguT_d = din("bguT", [128, NE, 16])
    w_down_d = din("w_down", [NE, D, D])
    bd_row_d = din("bd_row", [NE, D])
    fg_row_d = din("fg_row", [1, D])
    U_d = din("cU", [128, 128])
    L_d = din("cL", [128, 128])
    NEGF_d = din("cNEGF", [128, 128])
    NEGB_d = din("cNEGB", [128, 128])
    IDF_d = din("cIDF", [128, 128])
    out_d = nc.dram_tensor("out", [NB, SEQ, D], F32, kind="ExternalOutput").ap()

    sz_d = dscr("sz_s", [NB, SEQ, D], BF16)
    yscT_d = dscr("yscT_s", [NB, 8, 128, SEQ], BF16)
    yssdT_d = dscr("yssdT_s", [NB, 8, 128, SEQ], BF16)
    x1_d = dscr("x1_s", [NB, SEQ, D], F32)
    h2tok_d = dscr("h2tok_s", [NB * SEQ, D], BF16)
    NROWS = MNBLK * MBS
    xsorted_d = dscr("xsorted_s", [NROWS, D], BF16)
    ysorted_d = dscr("ysorted_s", [NROWS, D], F32)
    bguFM_d = din("bguFM", [NE * 128, 16])
    SU_d = din("cSU", [128, 128])
    PK_d = din("cPK", [128, 8])
    if debug:
        dbg_route = dscr("dbg_route", [128, NB * NT * 8 + 128], F32)
    if debug:
        dbg_xs = dscr("dbg_xs", [NB, 128, NT * D], BF16)
        dbg_dt = dscr("dbg_dt", [NB, 128, NT * 32], F32)
        dbg_h0 = dscr("dbg_h0", [NB, 2, 128, D], F32)
        dbg_gates = dscr("dbg_gates", [NB, 128, NT * NE], F32)
        dbg_hT = dscr("dbg_hT", [NB, 128, 8 * (SEQ + 2 * HP)], BF16)
        dbg_BT = dscr("dbg_BT", [NB, 2, 128, 2 * SEQ], BF16)

    w_in_v = w_in_d.rearrange("(kc p) n -> p kc n", p=128)
    w_mod_v = w_mod_d.rearrange("(kc p) n -> p kc n", p=128)
    w_out_v = w_out_d.rearrange("(kc p) n -> p kc n", p=128)
    w_router_v = w_router_d.rearrange("(kc p) n -> p kc n", p=128)

    with ExitStack() as st:
        AR = 52992
        arena_t = st.enter_context(nc.sbuf_tensor("arena", [128, AR], F32))
        A = Arena(arena_t[:, :], AR)
        psf = [st.enter_context(nc.psum_tensor("psf%d" % i, [128, 512], F32)) for i in range(6)]
        ptb = [st.enter_context(nc.psum_tensor("ptb%d" % i, [128, 1024], BF16)) for i in range(2)]
        P = Prog(nc)
        psc = [0]
        psn = [5]
        ukc = [0]

        def uk():
            ukc[0] += 1
            return "uk%d" % ukc[0]

        def nextps():
            i = psc[0] % psn[0]
            psc[0] += 1
            return psf[i][:, :], "ps%d" % i

        ptc = [0]

        def nextpt():
            i = ptc[0] % 2
            ptc[0] += 1
            return ptb[i][:, :], "pt%d" % i

        def mm(out, lhsT, rhs, start, stop, R, W):
            P.op("pe", lambda e: e.matmul(out, lhsT=lhsT, rhs=rhs, start=start, stop=stop), R, W)

        def tr(out, in_, ident, R, W):
            P.op("pe", lambda e: e.transpose(out, in_, ident), R, W)

        def tt(eng, out, in0, in1, op, R, W):
            P.op(eng, lambda e: e.tensor_tensor(out=out, in0=in0, in1=in1, op=op), R, W)

        def ts(eng, out, in0, s1, s2, op0, op1, R, W):
            if op1 is None:
                P.op(eng, lambda e: e.tensor_scalar(out=out, in0=in0, scalar1=s1, scalar2=None, op0=op0), R, W)
            else:
                P.op(eng, lambda e: e.tensor_scalar(out=out, in0=in0, scalar1=s1, scalar2=s2, op0=op0, op1=op1), R, W)

        def stt(eng, out, in0, scalar, in1, op0, op1, R, W):
            P.op(eng, lambda e: e.scalar_tensor_tensor(out=out, in0=in0, scalar=scalar, in1=in1, op0=op0, op1=op1), R, W)

        def act(out, in_, func, R, W, bias=None, scale=None, accum=None):
            kw = {}
            if bias is not None:
                kw["bias"] = bias
            if scale is not None:
                kw["scale"] = scale
            if accum is not None:
                kw["accum_out"] = accum
            P.op("act", lambda e: e.activation(out=out, in_=in_, func=func, **kw), R, W)

        def cp(eng, out, in_, R, W):
            if eng == "act":
                P.op("act", lambda e: e.copy(out=out, in_=in_), R, W)
            else:
                P.op(eng, lambda e: e.tensor_copy(out=out, in_=in_), R, W)

        def memset(eng, ap, val, W):
            P.op(eng, lambda e: e.memset(ap, val), (), W)

        def recip(out, in_, R, W):
            P.op("dve", lambda e: e.reciprocal(out=out, in_=in_), R, W)

        def dma(q, out, in_, sem, R, W, output=False, background=False):
            P.dma(q, lambda e: e.dma_start(out=out, in_=in_), sem, R, W, is_output=output, background=background)

        def rstd_from_ss(rstd, ss, n, key):
            ts("dve", rstd, ss, 1.0 / n, EPS, ALU.mult, ALU.add, [key + "ss"], [key + "rs"])
            act(rstd, rstd, AF.Sqrt, [key + "rs"], [key + "rs"])
            recip(rstd, rstd, [key + "rs"], [key + "rs"])

        ones_bf = A.bf16(128)
        bguT = A.f32(NE * 16).rearrange("p (e c) -> p e c", e=NE)
        gates_all = A.f32(NB * NT * NE).rearrange("p (t e) -> p t e", e=NE)
        gates = [gates_all[:, b_ * NT:(b_ + 1) * NT, :] for b_ in range(NB)]
        g2_bc = [A.f32(D) for _ in range(NB)]
        fg_bc = A.f32(D)
        IDB = A.bf16(128)
        G2 = A.f32(NB * 8).rearrange("p (v k) -> p v k", v=NB)
        S2 = A.f32(NB * 8).rearrange("p (v k) -> p v k", v=NB)
        perm_mark = A.mark()

        U = A.f32(128)
        L = A.f32(128)
        NEGF = A.f32(128)
        NEGB = A.f32(128)
        IDF = A.f32(128)
        ONESF = A.f32(128)
        G1 = A.f32(3 * 8).rearrange("p (v k) -> p v k", v=3)
        S1 = A.f32(3 * 8).rearrange("p (v k) -> p v k", v=3)
        dtb_bc = A.f32(32)
        A_bc = A.f32(32)
        D_bc = A.f32(16)
        convbT = A.f32(12)
        scwT = A.f32(24).rearrange("p (j k) -> p j k", j=8)
        br_bc = A.f32(NE)
        wr_f = A.f32(8 * NE).rearrange("p (k n) -> p k n", k=8)
        gssd_bc = A.f32(D)
        g1_bc = [A.f32(D) for _ in range(NB)]
        H0 = [A.f32(D) for _ in range(2)]
        convb_bf = A.bf16(1536)
        zt = A.bf16(2048)
        mixc_mark = A.mark()

        def chk(name):
            if stop_after == name:
                raise _Stop()

        def _body():
            for (t, src, k) in [(U, U_d, "U"), (L, L_d, "L"), (NEGF, NEGF_d, "NEGF"), (NEGB, NEGB_d, "NEGB"), (IDF, IDF_d, "IDF")]:
                dma("sp", t, src, "c_" + k, [], [k])
            dma("pool", IDB, IDF_d, "c_IDB", [], ["IDB"])
            memset("dve", ONESF, 1.0, ["ONESF"])
            memset("dve", ones_bf, 1.0, ["ones_bf"])
            dma("sp", bguT, bguT_d, "c_bgu", [], ["bguT"])
            ts("dve", bguT[:, :, 8:16], bguT[:, :, 8:16], 1.0, None, ALU.add, None, ["bguT"], ["bguT"])
            dma("sp", fg_bc, fg_row_d.partition_broadcast(128), "c_fg", [], ["fg_bc"])
            dma("sp", gssd_bc, gssd_row_d.partition_broadcast(128), "c_gssd", [], ["gssd_bc"])
            dma("sp", dtb_bc, dtb_row_d.partition_broadcast(128), "c_dtb", [], ["dtb_bc"])
            dma("sp", A_bc, alog_row_d.partition_broadcast(128), "c_alog", [], ["A_bc"])
            act(A_bc, A_bc, AF.Exp, ["A_bc"], ["A_bc"])
            ts("dve", A_bc, A_bc, -1.0, None, ALU.mult, None, ["A_bc"], ["A_bc"])
            dma("sp", D_bc, d_row_d.partition_broadcast(128), "c_d", [], ["D_bc"])
            dma("sp", convbT, convbT_d, "c_cbT", [], ["convbT"])
            dma("sp", scwT, scwT_d, "c_scw", [], ["scwT"])
            dma("sp", br_bc, br_row_d.partition_broadcast(128), "c_br", [], ["br_bc"])
            dma("sp", wr_f, w_router_v, "c_wr", [], ["wr_f"])
            dma("pool", convb_bf[0:1, :], convb_row_d, "c_cbb", [], ["convb_bf"])

            m0 = A.mark()
            cT_f = A.f32(24).rearrange("p (k v) -> p k v", k=8)
            scT_bf = A.bf16(24).rearrange("p (k v) -> p k v", k=8)
            scB = [A.bf16(8 * 128).rearrange("p (k m) -> p k m", k=8) for _ in range(NB)]
            b_modT = A.f32(48)
            n1gT = A.f32(8)
            n2gT = A.f32(8)
            modT = A.f32(48 * 3).rearrange("p (o v) -> p o v", o=48)
            bmod_bc = [A.f32(D) for _ in range(2)]
            wm = [A.bf16(8 * 128).rearrange("p (k n) -> p k n", k=8) for _ in range(3)]
            dma("sp", cT_f, cT_d, "m_cT", [], ["cT_f"])
            dma("sp", b_modT, b_modT_d, "m_bmT", [], ["b_modT"])
            dma("sp", n1gT, n1gT_d, "m_n1", [], ["n1gT"])
            dma("sp", n2gT, n2gT_d, "m_n2", [], ["n2gT"])
            dma("sp", bmod_bc[0], b_mod_row_d[:, 2 * D:3 * D].partition_broadcast(128), "m_bb0", [], ["bmod_bc0"])
            dma("sp", bmod_bc[1], b_mod_row_d[:, 5 * D:6 * D].partition_broadcast(128), "m_bb1", [], ["bmod_bc1"])
            act(scT_bf, cT_f, AF.Silu, ["cT_f"], ["scT_bf"])
            for v in range(NB):
                cp("dve", scB[v], scT_bf[:, :, v:v + 1].to_broadcast([128, 8, 128]), ["scT_bf"], ["scB%d" % v])
            for oc in range(48):
                mod, cc = oc // 8, oc % 8
                s = oc % 3
                dma("pool", wm[s], w_mod_v[:, :, oc * 128:(oc + 1) * 128], "m_wm%d" % s, [], ["wm%d" % s])
                if mod in (0, 1, 3, 4):
                    ps, pk = nextps()
                    for kc in range(8):
                        mm(ps[:, 0:3], wm[s][:, kc, :], scT_bf[:, kc, :], kc == 0, kc == 7, ["wm%d" % s, "scT_bf"], [pk])
                    ts("dve", modT[:, oc, :], ps[:, 0:3], b_modT[:, oc:oc + 1], None, ALU.add, None, [pk, "b_modT"], ["modT"])
                else:
                    gi = 0 if mod == 2 else 1
                    for v in range(NB):
                        ps, pk = nextps()
                        for kc in range(8):
                            mm(ps[:, 0:128], scB[v][:, kc, :], wm[s][:, kc, :], kc == 0, kc == 7, ["wm%d" % s, "scB%d" % v], [pk])
                        dst = (g1_bc if mod == 2 else g2_bc)[v]
                        tt("dve", dst[:, cc * 128:(cc + 1) * 128], ps[:, 0:128], bmod_bc[gi][:, cc * 128:(cc + 1) * 128], ALU.add,
                           [pk, "bmod_bc%d" % gi], ["g%d_bc%d" % (1 if mod == 2 else 2, v)])
            for v in range(3):
                stt("dve", G1[:, v, :], modT[:, 8:16, v], 1.0, n1gT, ALU.add, ALU.mult, ["modT", "n1gT"], ["G1"])
                cp("dve", S1[:, v, :], modT[:, 0:8, v], ["modT"], ["S1"])
            for v in range(NB):
                stt("dve", G2[:, v, :], modT[:, 32:40, v], 1.0, n2gT, ALU.add, ALU.mult, ["modT", "n2gT"], ["G2"])
                cp("dve", S2[:, v, :], modT[:, 24:32, v], ["modT"], ["S2"])
            P.barrier()
            A.reset(m0)
            chk("phase0")

            def norm_mod_T(src_rows, ntiles, Gv, Sv, hT, hkey, tag):
                mk = A.mark()
                xt = [A.f32(D) for _ in range(2)]
                xn = [A.bf16(D) for _ in range(2)]
                junk = A.f32(D)
                tmp = [A.f32(D).rearrange("p (k t) -> p k t", k=8) for _ in range(2)]
                ss = A.f32(4)
                rs = A.f32(4)
                memset("dve", hT[:, :, 0:HP], 0.0, [hkey])
                memset("dve", hT[:, :, ntiles * 128 + HP:ntiles * 128 + 2 * HP], 0.0, [hkey])
                for t in range(ntiles):
                    i = t % 2
                    k = "%s%d" % (tag, i)
                    dma("sp", xt[i], src_rows(t), "nm_x%d" % i, [], [k + "xt"])
                    memset("dve", ss[:, i:i + 1], 0.0, [k + "ss"])
                    act(junk, xt[i], AF.Square, [k + "xt"], [tag + "junk", k + "ss"], accum=ss[:, i:i + 1])
                    rstd_from_ss(rs[:, i:i + 1], ss[:, i:i + 1], D, k)
                    ts("dve", xn[i], xt[i], rs[:, i:i + 1], None, ALU.mult, None, [k + "xt", k + "rs"], [k + "xn"])
                    pt, ptk = nextpt()
                    ptv = pt.rearrange("p (k t) -> p k t", k=8)
                    for kc in range(8):
                        tr(ptv[:, kc, :], xn[i][:, kc * 128:(kc + 1) * 128], IDB, [k + "xn", "IDB"], [ptk])
                    tt("dve", tmp[i], ptv, Gv.unsqueeze(2).to_broadcast([128, 8, 128]), ALU.mult, [ptk, "G1", "G2"], [k + "tmp"])
                    tt("pool", hT[:, :, HP + t * 128:HP + (t + 1) * 128], tmp[i], Sv.unsqueeze(2).to_broadcast([128, 8, 128]), ALU.add,
                       [k + "tmp", "S1", "S2"], [hkey])
                P.barrier()
                A.reset(mk)

            def inproj_tok(hT, hkey, ntiles, col0, W, conv, evac, ring, tag):
                wr, wk, cw = ring
                dma("pool", wr[:, :, 0:W], w_in_v[:, :, col0:col0 + W], "ip_wr", [], ["ip_wr"])
                if conv:
                    c0 = col0 - X0
                    for k in range(3):
                        dma("sp", cw[:, k, 0:W], convw_d[k:k + 1, c0:c0 + W].partition_broadcast(128), "ip_cw%d" % k, [], ["ip_cw%d" % k])
                        tt("dve" if k != 1 else "pool", wk[k][:, :, 0:W], wr[:, :, 0:W], cw[:, k, 0:W].unsqueeze(1).to_broadcast([128, 8, W]), ALU.mult,
                           ["ip_wr", "ip_cw%d" % k], ["ip_wk%d" % k])
                for t in range(ntiles):
                    ps, pk = nextps()
                    if conv:
                        n = 0
                        for k in range(3):
                            for kc in range(8):
                                mm(ps[:, 0:W], hT[:, kc, HP - 1 + t * 128 + k:HP - 1 + t * 128 + k + 128], wk[k][:, kc, 0:W], n == 0, False,
                                   [hkey, "ip_wk%d" % k], [pk])
                                n += 1
                        mm(ps[:, 0:W], ones_bf[0:1, 0:128], convb_bf[0:1, c0:c0 + W], False, True, ["ones_bf", "convb_bf"], [pk])
                    else:
                        for kc in range(8):
                            mm(ps[:, 0:W], hT[:, kc, HP + t * 128:HP + (t + 1) * 128], wr[:, kc, 0:W], kc == 0, kc == 7, [hkey, "ip_wr"], [pk])
                    evac(t, ps[:, 0:W], pk)

            def softplus_dt(dt_all, n, key):
                mk = A.mark()
                ta = A.f32(n)
                stt("dve", ta, dt_all, -1.0, dt_all, ALU.mult, ALU.max, [key], [key + "ta"])
                act(ta, ta, AF.Exp, [key + "ta"], [key + "ta"], scale=-1.0)
                act(ta, ta, AF.Ln, [key + "ta"], [key + "ta"], bias=1.0)
                stt("dve", dt_all, dt_all, 0.0, ta, ALU.max, ALU.add, [key, key + "ta"], [key])
                P.barrier()
                A.reset(mk)

            def ssd_scalars(dt_all, ntiles, bufs, key):
                dtA, acs, e_all, nacs, cd, wdec = bufs
                n = ntiles * 32
                v3 = lambda a: a.rearrange("p (t c) -> p t c", c=32)
                tt("dve", v3(dtA), v3(dt_all), A_bc.unsqueeze(1).to_broadcast([128, ntiles, 32]), ALU.mult, [key, "A_bc"], [key + "dtA"])
                ps1, pk1 = nextps()
                mm(ps1[:, 0:n], U, dtA, True, True, ["U", key + "dtA"], [pk1])
                cp("dve", v3(acs)[:, :, 0:16], v3(ps1[:, 0:n])[:, :, 0:16], [pk1], [key + "acs"])
                chk("sc1")
                ps2, pk2 = nextps()
                mm(ps2[:, 0:n], L, dtA, True, True, ["L", key + "dtA"], [pk2])
                cp("dve", v3(acs)[:, :, 16:32], v3(ps2[:, 0:n])[:, :, 16:32], [pk2], [key + "acs"])
                ps3, pk3 = nextps()
                mm(ps3[:, 0:n], ONESF, dtA, True, True, ["ONESF", key + "dtA"], [pk3])
                chk("sc2")
                act(e_all, acs, AF.Exp, [key + "acs"], [key + "e"])
                ts("dve", nacs, acs, -1.0, None, ALU.mult, None, [key + "acs"], [key + "nacs"])
                act(cd, ps3[:, 0:n], AF.Exp, [pk3], [key + "cd"])
                chk("sc3")
                tt("dve", wdec, ps3[:, 0:n], nacs, ALU.add, [pk3, key + "nacs"], [key + "wdec"])
                chk("sc4")
                act(wdec, wdec, AF.Exp, [key + "wdec"], [key + "wdec"])
                chk("sc5")
                tt("dve", wdec, wdec, dt_all, ALU.mult, [key + "wdec", key], [key + "wdec"])

            def bc64(ap16, nh=16):
                return ap16.unsqueeze(2).to_broadcast([128, nh, 64])

            def hv(ap, nh=16):
                return ap.rearrange("p (h d) -> p h d", h=nh)

            def state_update(H, Hkey, xs_tile, xs_key, Btok_tile, B_key, wdec16, cd16, sc_key, xdec, tag):
                tt("dve", hv(xdec), hv(xs_tile), bc64(wdec16), ALU.mult, [xs_key, sc_key + "wdec"], [tag + "xdec"])
                tt("dve", hv(H), hv(H), bc64(cd16), ALU.mult, [Hkey, sc_key + "cd"], [Hkey])
                for g in range(2):
                    ps, pk = nextps()
                    mm(ps, Btok_tile[:, g * 128:(g + 1) * 128], xdec[:, g * 512:(g + 1) * 512], True, True, [B_key, tag + "xdec"], [pk])
                    tt("dve", H[:, g * 512:(g + 1) * 512], H[:, g * 512:(g + 1) * 512], ps, ALU.add, [Hkey, pk], [Hkey])

            for b in range(NB):
                bm = A.mark()
                xs = A.bf16(NT * D).rearrange("p (t c) -> p t c", t=NT)
                Btok = A.bf16(NT * 256).rearrange("p (t c) -> p t c", t=NT)
                BT = A.bf16(2 * SEQ).rearrange("p (g t) -> p g t", g=2)
                CT = A.bf16(2 * SEQ).rearrange("p (g t) -> p g t", g=2)
                dt_all = A.f32(NT * 32)
                scal = [A.f32(NT * 32) for _ in range(6)]
                dtA, acs, e_all, nacs, cd, wdec = scal
                v3 = lambda a: a.rearrange("p (t c) -> p t c", c=32)

                cm = A.mark()
                hcT = A.bf16(8 * (CTX + 2 * HP)).rearrange("p (k t) -> p k t", k=8)
                ring = (A.bf16(8 * 256).rearrange("p (k n) -> p k n", k=8),
                        [A.bf16(8 * 256).rearrange("p (k n) -> p k n", k=8) for _ in range(3)],
                        A.f32(3 * 256).rearrange("p (k n) -> p k n", k=3))
                norm_mod_T(lambda t: ctx_d[b, t * 128:(t + 1) * 128, :], 2, G1[:, 2, :], S1[:, 2, :], hcT, "hcT", "cn")
                chk("ctx_a")
                for gq in range(4):
                    inproj_tok(hcT, "hcT", 2, X0 + gq * 256, 256, True,
                               lambda t, ps, pk, gq=gq: act(xs[:, t, gq * 256:(gq + 1) * 256], ps, AF.Silu, [pk], ["xs"]), ring, "c")
                chk("ctx_b")
                inproj_tok(hcT, "hcT", 2, B0, 256, True, lambda t, ps, pk: act(Btok[:, t, :], ps, AF.Silu, [pk], ["Btok"]), ring, "c")
                inproj_tok(hcT, "hcT", 2, DT0, 32, False,
                           lambda t, ps, pk: tt("dve", dt_all[:, t * 32:(t + 1) * 32], ps, dtb_bc, ALU.add, [pk, "dtb_bc"], ["dt_all"]), ring, "c")
                P.barrier()
                chk("ctx_c")
                softplus_dt(dt_all[:, 0:64], 64, "dt_all")
                chk("ctx_d")
                ssd_scalars(dt_all[:, 0:64], 2, [a[:, 0:64] for a in scal], "dt_all")
                chk("ctx_e")
                xdec = A.bf16(D)
                memset("dve", H0[0], 0.0, ["H0f"])
                memset("dve", H0[1], 0.0, ["H0b"])
                for c in (0, 1):
                    state_update(H0[0], "H0f", xs[:, c, :], "xs", Btok[:, c, :], "Btok", v3(wdec)[:, c, 0:16], v3(cd)[:, c, 0:16], "dt_all", xdec, "cf")
                for c in (1, 0):
                    state_update(H0[1], "H0b", xs[:, c, :], "xs", Btok[:, c, :], "Btok", v3(wdec)[:, c, 16:32], v3(cd)[:, c, 16:32], "dt_all", xdec, "cb")
                if debug:
                    dma("sp", dbg_h0[b, 0], H0[0], "dbg0", ["H0f"], ["dbg_h0"])
                    dma("sp", dbg_h0[b, 1], H0[1], "dbg0", ["H0b"], ["dbg_h0"])
                P.barrier()
                A.reset(cm)
                chk("ctx")

                pm_ = A.mark()
                hT = A.bf16(8 * (SEQ + 2 * HP)).rearrange("p (k t) -> p k t", k=8)
                norm_mod_T(lambda t: x_d[b, t * 128:(t + 1) * 128, :], NT, G1[:, b, :], S1[:, b, :], hT, "hT", "xn")
                if debug:
                    dma("sp", dbg_hT[b], hT.rearrange("p k t -> p (k t)"), "dbg1", ["hT"], ["dbg_hT"])

                chk("p1")
                p2m = A.mark()
                ring = (A.bf16(8 * 256).rearrange("p (k n) -> p k n", k=8),
                        [A.bf16(8 * 256).rearrange("p (k n) -> p k n", k=8) for _ in range(3)],
                        A.f32(3 * 256).rearrange("p (k n) -> p k n", k=3))
                szt = [A.bf16(256) for _ in range(4)]
                szc = [0]

                def evac_z(t, ps, pk, gq):
                    i = szc[0] % 4
                    szc[0] += 1
                    act(szt[i], ps, AF.Silu, [pk], ["szt%d" % i])
                    dma("sp", sz_d[b, t * 128:(t + 1) * 128, gq * 256:(gq + 1) * 256], szt[i], "szo%d" % i, ["szt%d" % i], [uk()])

                for gq in range(4):
                    inproj_tok(hT, "hT", NT, gq * 256, 256, False, lambda t, ps, pk, gq=gq: evac_z(t, ps, pk, gq), ring, "l")
                for gq in range(4):
                    inproj_tok(hT, "hT", NT, X0 + gq * 256, 256, True,
                               lambda t, ps, pk, gq=gq: act(xs[:, t, gq * 256:(gq + 1) * 256], ps, AF.Silu, [pk], ["xs"]), ring, "l")
                inproj_tok(hT, "hT", NT, B0, 256, True, lambda t, ps, pk: act(Btok[:, t, :], ps, AF.Silu, [pk], ["Btok"]), ring, "l")
                inproj_tok(hT, "hT", NT, DT0, 32, False,
                           lambda t, ps, pk: tt("dve", dt_all[:, t * 32:(t + 1) * 32], ps, dtb_bc, ALU.add, [pk, "dtb_bc"], ["dt_all"]), ring, "l")
                wr, wk, cw = ring
                for q in range(4):
                    col0 = B0 + q * 128
                    c0 = col0 - X0
                    dst = (BT if q < 2 else CT)
                    dkey = "BT" if q < 2 else "CT"
                    g = q % 2
                    dma("pool", wr[:, :, 0:128], w_in_v[:, :, col0:col0 + 128], "ip_wr", [], ["ip_wr"])
                    for k in range(3):
                        dma("sp", cw[:, k, 0:128], convw_d[k:k + 1, c0:c0 + 128].partition_broadcast(128), "ip_cw%d" % k, [], ["ip_cw%d" % k])
                        tt("dve", wk[k][:, :, 0:128], wr[:, :, 0:128], cw[:, k, 0:128].unsqueeze(1).to_broadcast([128, 8, 128]), ALU.mult,
                           ["ip_wr", "ip_cw%d" % k], ["ip_wk%d" % k])
                    for tg in range(4):
                        ps, pk = nextps()
                        n = 0
                        for k in range(3):
                            for kc in range(8):
                                mm(ps, wk[k][:, kc, 0:128], hT[:, kc, HP - 1 + tg * 512 + k:HP - 1 + tg * 512 + k + 512], n == 0, n == 23, ["hT", "ip_wk%d" % k], [pk])
                                n += 1
                        act(dst[:, g, tg * 512:(tg + 1) * 512], ps, AF.Silu, [pk, "convbT"], [dkey], bias=convbT[:, 8 + q:9 + q])
                P.barrier()
                A.reset(p2m)
                wsc = [A.bf16(3 * 8 * 128).rearrange("p (a k n) -> p a k n", a=3, k=8) for _ in range(2)]
                c_sb = [A.f32(512) for _ in range(2)]
                vbufs = [A.bf16(SEQ + 128) for _ in range(2)]
                b_sbs = [A.bf16(SEQ) for _ in range(2)]
                t1 = A.f32(SEQ)
                ysc = [A.bf16(SEQ) for _ in range(2)]
                for s_ in range(2):
                    memset("dve", vbufs[s_][:, 0:64], 0.0, ["vbuf%d" % s_])
                    memset("dve", vbufs[s_][:, SEQ + 64:SEQ + 128], 0.0, ["vbuf%d" % s_])
                for j in range(8):
                    s = j % 2
                    vbuf, b_sb = vbufs[s], b_sbs[s]
                    for a in range(3):
                        cs = SC0 + a * D + j * 128
                        dma("pool", wsc[s][:, a, :, :], w_in_v[:, :, cs:cs + 128], "wsc%d" % s, [], ["wsc%d" % s])
                    for tg in range(4):
                        pss = []
                        for a in range(3):
                            ps, pk = nextps()
                            for kc in range(8):
                                mm(ps, wsc[s][:, a, kc, :], hT[:, kc, HP + tg * 512:HP + (tg + 1) * 512], kc == 0, kc == 7, ["hT", "wsc%d" % s], [pk])
                            pss.append((ps, pk))
                        i = tg % 2
                        cp("act", b_sb[:, tg * 512:(tg + 1) * 512], pss[0][0], [pss[0][1]], ["b_sb%d" % s])
                        cp("act", c_sb[i], pss[1][0], [pss[1][1]], ["c_sb%d" % i])
                        tt("dve", vbuf[:, 64 + tg * 512:64 + (tg + 1) * 512], pss[2][0], c_sb[i], ALU.mult, [pss[2][1], "c_sb%d" % i], ["vbuf%d" % s])
                    ts("dve", t1, vbuf[:, 0:SEQ], scwT[:, j, 0:1], None, ALU.mult, None, ["vbuf%d" % s, "scwT"], ["sc_t1"])
                    stt("dve", t1, vbuf[:, 64:64 + SEQ], scwT[:, j, 1:2], t1, ALU.mult, ALU.add, ["vbuf%d" % s, "scwT", "sc_t1"], ["sc_t1"])
                    stt("dve", t1, vbuf[:, 128:128 + SEQ], scwT[:, j, 2:3], t1, ALU.mult, ALU.add, ["vbuf%d" % s, "scwT", "sc_t1"], ["sc_t1"])
                    tt("dve", ysc[s], t1, b_sb, ALU.mult, ["sc_t1", "b_sb%d" % s], ["ysc%d" % s])
                    dma("sp", yscT_d[b, j], ysc[s], "ysco%d" % s, ["ysc%d" % s], [uk()])
                P.barrier()
                A.reset(pm_)
                if debug:
                    dma("sp", dbg_xs[b], xs.rearrange("p t c -> p (t c)"), "dbg2", ["xs"], ["dbg_xs"])
                    dma("sp", dbg_BT[b, 0], BT.rearrange("p g t -> p (g t)"), "dbg3", ["BT"], ["dbg_BT"])
                    dma("sp", dbg_BT[b, 1], CT.rearrange("p g t -> p (g t)"), "dbg3", ["CT"], ["dbg_BT"])

                chk("p2")
                softplus_dt(dt_all, NT * 32, "dt_all")
                for t0 in range(0, NT, 2):
                    ssd_scalars(dt_all[:, t0 * 32:(t0 + 2) * 32], 2, [a[:, t0 * 32:(t0 + 2) * 32] for a in scal], "dt_all")
                if debug:
                    dma("sp", dbg_dt[b], dt_all, "dbg4", ["dt_all"], ["dbg_dt"])

                if b == 0:
                    memset("dve", zt, 0.0, ["zt"])
                sm = A.mark()
                Hb_all = A.bf16(NT * D).rearrange("p (t c) -> p t c", t=NT)
                Hb = A.f32(D)
                Hf = A.f32(D)
                Hf_bf = A.bf16(D)
                xdec = A.bf16(D)
                xdt = [A.bf16(D) for _ in range(2)]
                xsD = A.bf16(D)
                cp("dve", Hb, H0[1], ["H0b"], ["Hb"])
                for c in range(NT - 1, -1, -1):
                    cp("act", Hb_all[:, c, :], Hb, ["Hb"], ["Hb_all%d" % c])
                    state_update(Hb, "Hb", xs[:, c, :], "xs", Btok[:, c, :], "Btok", v3(wdec)[:, c, 16:32], v3(cd)[:, c, 16:32], "dt_all", xdec, "bs")

                chk("bsweep")
                CBT = [A.f32(128) for _ in range(2)]
                Eb = [A.f32(128) for _ in range(6)]
                MTb = [A.bf16(128) for _ in range(6)]
                ytile = A.f32(D)
                tA = A.f32(512)
                tB = A.f32(512)
                szl = [A.bf16(D) for _ in range(2)]
                yg = A.f32(D)
                junk = A.f32(512)
                ss = A.f32(2)
                rs = A.f32(2)
                ymix = A.bf16(D)
                ymT = [A.bf16(D).rearrange("p (k t) -> p k t", k=8) for _ in range(2)]
                cp("dve", Hf, H0[0], ["H0f"], ["Hf"])
                cp("act", Hf_bf, Hf, ["Hf"], ["Hf_bf"])
                ec = [0]
                units = [(c_, g_, hh_, d_) for c_ in range(NT) for g_ in range(2) for hh_ in range(8) for d_ in range(2)]
                LEAD = 3
                cb_done = set()
                slot_of = {}
                psn[0] = 4

                def ensureCB(c_, g_):
                    if (c_, g_) in cb_done:
                        return
                    cb_done.add((c_, g_))
                    psb, pkb = nextps()
                    mm(psb[:, 0:128], BT[:, g_, c_ * 128:(c_ + 1) * 128], CT[:, g_, c_ * 128:(c_ + 1) * 128], True, True, ["BT", "CT"], [pkb])
                    cp("act", CBT[g_], psb[:, 0:128], [pkb], ["CBT%d" % g_])

                def emitA(ui):
                    c_, g_, hh_, d_ = units[ui]
                    ensureCB(c_, g_)
                    h_ = g_ * 8 + hh_
                    col = d_ * 16 + h_
                    psr, pkr = nextps()
                    mm(psr[:, 0:128], v3(dtA)[:, c_, col:col + 1].to_broadcast([128, 128]), U if d_ == 0 else L, True, False,
                       ["dt_alldtA", "U", "L"], [pkr])
                    mm(psr[:, 0:128], IDF, NEGF if d_ == 0 else NEGB, False, True, ["IDF", "NEGF", "NEGB"], [pkr])
                    ei = ec[0] % 6
                    ec[0] += 1
                    slot_of[ui] = ei
                    act(Eb[ei], psr[:, 0:128], AF.Exp, [pkr, "dt_allnacs"], ["E%d" % ei], bias=v3(nacs)[:, c_, col:col + 1])
                    tt("dve", MTb[ei], Eb[ei], CBT[g_], ALU.mult, ["E%d" % ei, "CBT%d" % g_], ["MT%d" % ei])

                def emitB(ui, psy, pky):
                    c_, g_, hh_, d_ = units[ui]
                    h_ = g_ * 8 + hh_
                    ei = slot_of[ui]
                    mm(psy[:, hh_ * 64:(hh_ + 1) * 64], MTb[ei], xdt[d_][:, h_ * 64:(h_ + 1) * 64], False, (hh_ == 7 and d_ == 1),
                       ["MT%d" % ei, "xdt%d" % d_], [pky])

                for ui in range(LEAD):
                    emitA(ui)
                xs_v = xsorted_d.rearrange("(p r) n -> p (r n)", p=128)
                ZW = 1536
                nz = (NROWS // 128) * D // ZW
                zper = -(-nz // NT)
                for c in range(NT):
                    ci = c % 2
                    if b == 0:
                        for zi in range(c * zper, min((c + 1) * zper, nz)):
                            dma("sp", xs_v[:, zi * ZW:(zi + 1) * ZW], zt[:, 0:ZW], "zfill", ["zt"],
                                (["xsorted_d"] if zi == nz - 1 else []) + ["zf%d" % (zi % 8)], background=True)
                    dma("sp", szl[ci], sz_d[b, c * 128:(c + 1) * 128, :], "szl%d" % ci, ["sz_d"], ["szl%d" % ci])
                    tt("dve", hv(xdt[0]), hv(xs[:, c, :]), bc64(v3(dt_all)[:, c, 0:16]), ALU.mult, ["xs", "dt_all"], ["xdt0"])
                    tt("pool", hv(xdt[1]), hv(xs[:, c, :]), bc64(v3(dt_all)[:, c, 16:32]), ALU.mult, ["xs", "dt_all"], ["xdt1"])
                    tt("pool", hv(xsD), hv(xs[:, c, :]), bc64(D_bc), ALU.mult, ["xs", "D_bc"], ["xsD"])
                    tt("dve", hv(xdec), hv(xs[:, c, :]), bc64(v3(wdec)[:, c, 0:16]), ALU.mult, ["xs", "dt_allwdec"], ["fsxdec"])
                    for g in range(2):
                        psy, pky = (psf[5][:, :], "ps5") if g == 0 else (psf[4][:, :], "ps4")
                        mm(psy, IDB, xsD[:, g * 512:(g + 1) * 512], True, False, ["IDB", "xsD"], [pky])
                        for hh in range(8):
                            for d in range(2):
                                ui = ((c * 2 + g) * 8 + hh) * 2 + d
                                emitB(ui, psy, pky)
                                if ui + LEAD < len(units):
                                    emitA(ui + LEAD)
                        pof, pkf = nextps()
                        mm(pof, CT[:, g, c * 128:(c + 1) * 128], Hf_bf[:, g * 512:(g + 1) * 512], True, True, ["CT", "Hf_bf"], [pkf])
                        pob, pkb2 = nextps()
                        mm(pob, CT[:, g, c * 128:(c + 1) * 128], Hb_all[:, c, g * 512:(g + 1) * 512], True, True, ["CT", "Hb_all%d" % c], [pkb2])
                        tt("dve", hv(tA, 8), hv(pof, 8), bc64(v3(e_all)[:, c, g * 8:(g + 1) * 8], 8), ALU.mult, [pkf, "dt_alle"], ["tA"])
                        tt("dve", hv(tB, 8), hv(pob, 8), bc64(v3(e_all)[:, c, 16 + g * 8:16 + (g + 1) * 8], 8), ALU.mult, [pkb2, "dt_alle"], ["tB"])
                        tt("pool", tA, tA, tB, ALU.add, ["tA", "tB"], ["tA"])
                        tt("dve", ytile[:, g * 512:(g + 1) * 512], psy, tA, ALU.add, [pky, "tA"], ["ytile"])
                    tt("dve", hv(Hf), hv(Hf), bc64(v3(cd)[:, c, 0:16]), ALU.mult, ["Hf", "dt_allcd"], ["Hf"])
                    for g in range(2):
                        ps, pk = nextps()
                        mm(ps, Btok[:, c, g * 128:(g + 1) * 128], xdec[:, g * 512:(g + 1) * 512], True, True, ["Btok", "fsxdec"], [pk])
                        tt("dve", Hf[:, g * 512:(g + 1) * 512], Hf[:, g * 512:(g + 1) * 512], ps, ALU.add, ["Hf", pk], ["Hf"])
                    cp("act", Hf_bf, Hf, ["Hf"], ["Hf_bf"])
                    tt("dve", yg, ytile, szl[ci], ALU.mult, ["ytile", "szl%d" % ci], ["yg"])
                    for g in range(2):
                        memset("dve", ss[:, g:g + 1], 0.0, ["gss"])
                        act(junk, yg[:, g * 512:(g + 1) * 512], AF.Square, ["yg"], ["gjunk", "gss"], accum=ss[:, g:g + 1])
                    rstd_from_ss(rs, ss, 512, "g")
                    for g in range(2):
                        stt("dve", ymix[:, g * 512:(g + 1) * 512], yg[:, g * 512:(g + 1) * 512], rs[:, g:g + 1], gssd_bc[:, g * 512:(g + 1) * 512],
                            ALU.mult, ALU.mult, ["yg", "grs", "gssd_bc"], ["ymix"])
                    pt, ptk = nextpt()
                    ptv = pt.rearrange("p (k t) -> p k t", k=8)
                    for kc in range(8):
                        tr(ptv[:, kc, :], ymix[:, kc * 128:(kc + 1) * 128], IDB, ["ymix", "IDB"], [ptk])
                    cp("act", ymT[ci], ptv, [ptk], ["ymT%d" % ci])
                    dma("sp", yssdT_d[b].rearrange("k p t -> p k t")[:, :, c * 128:(c + 1) * 128], ymT[ci], "ymo%d" % ci, ["ymT%d" % ci], [uk()])
                P.barrier()
                A.reset(bm)

                psn[0] = 5
                chk("sweep")
                w_out_bf = A.bf16(16 * D).rearrange("p (k n) -> p k n", k=16)
                ymg = [A.bf16(16 * 512).rearrange("p (k t) -> p k t", k=16) for _ in range(2)]
                xt = [A.f32(D) for _ in range(2)]
                x1 = [A.f32(D) for _ in range(2)]
                tPs = [A.f32(D) for _ in range(2)]
                junk = A.f32(D)
                ss = A.f32(2)
                rs = A.f32(2)
                xn2s = [A.bf16(D) for _ in range(2)]
                tmp = A.f32(D).rearrange("p (k t) -> p k t", k=8)
                h2f = A.f32(D).rearrange("p (k t) -> p k t", k=8)
                h2b = [A.bf16(D).rearrange("p (k t) -> p k t", k=8) for _ in range(2)]
                h2tok = [A.bf16(D) for _ in range(2)]
                lg = A.f32(NE)
                mx8 = A.f32(8)
                nmx = A.f32(1)
                msk = A.f32(NE)
                ex = A.f32(NE)
                sm_ = A.f32(1)
                for hf in range(2):
                    dma("pool", w_out_bf[:, hf * 8:(hf + 1) * 8, :], w_out_v[:, hf * 8:(hf + 1) * 8, :], "wout", [], ["w_out_bf"])
                def p4_s1(t):
                    tg, tl = t // 4, t % 4
                    gi = tg % 2
                    i = t % 2
                    if tl == 0:
                        dma("sp", ymg[gi][:, 0:8, :], yssdT_d[b].rearrange("k p t -> p k t")[:, :, tg * 512:(tg + 1) * 512], "ymg%d" % gi, ["yssdT_d"], ["ymg%d" % gi])
                        dma("sp", ymg[gi][:, 8:16, :], yscT_d[b].rearrange("k p t -> p k t")[:, :, tg * 512:(tg + 1) * 512], "ymgb%d" % gi, ["yscT_d"], ["ymgb%d" % gi])
                    dma("sp", xt[i], x_d[b, t * 128:(t + 1) * 128, :], "p4x%d" % i, [], ["p4xt%d" % i])
                    for hf in range(2):
                        ps, pk = nextps()
                        for kc in range(16):
                            mm(ps, ymg[gi][:, kc, tl * 128:(tl + 1) * 128], w_out_bf[:, kc, hf * 512:(hf + 1) * 512], kc == 0, kc == 15,
                               ["ymg%d" % gi, "ymgb%d" % gi, "w_out_bf"], [pk])
                        tt("dve", tPs[i][:, hf * 512:(hf + 1) * 512], ps, g1_bc[b][:, hf * 512:(hf + 1) * 512], ALU.mult, [pk, "g1_bc%d" % b], ["tP%d" % i])
                    tt("pool", x1[i], xt[i], tPs[i], ALU.add, ["p4xt%d" % i, "tP%d" % i], ["x1_%d" % i])
                    dma("sp", x1_d[b, t * 128:(t + 1) * 128, :], x1[i], "x1o%d" % i, ["x1_%d" % i], [uk()])
                    memset("dve", ss[:, i:i + 1], 0.0, ["p4%dss" % i])
                    act(junk, x1[i], AF.Square, ["x1_%d" % i], ["p4junk", "p4%dss" % i], accum=ss[:, i:i + 1])
                    rstd_from_ss(rs[:, i:i + 1], ss[:, i:i + 1], D, "p4%d" % i)
                    ts("dve", xn2s[i], x1[i], rs[:, i:i + 1], None, ALU.mult, None, ["x1_%d" % i, "p4%drs" % i], ["xn2_%d" % i])

                def p4_s2(t):
                    i = t % 2
                    pt, ptk = nextpt()
                    ptv = pt.rearrange("p (k t) -> p k t", k=8)
                    for kc in range(8):
                        tr(ptv[:, kc, :], xn2s[i][:, kc * 128:(kc + 1) * 128], IDB, ["xn2_%d" % i, "IDB"], [ptk])
                    tt("dve", tmp, ptv, G2[:, b, :].unsqueeze(2).to_broadcast([128, 8, 128]), ALU.mult, [ptk, "G2"], ["p4tmp"])
                    tt("pool", h2f, tmp, S2[:, b, :].unsqueeze(2).to_broadcast([128, 8, 128]), ALU.add, ["p4tmp", "S2"], ["h2f"])
                    cp("act", h2b[i], h2f, ["h2f"], ["h2b%d" % i])
                    pt2, ptk2 = nextpt()
                    ptv2 = pt2.rearrange("p (k t) -> p k t", k=8)
                    for kc in range(8):
                        tr(ptv2[:, kc, :], h2b[i][:, kc, :], IDB, ["h2b%d" % i, "IDB"], [ptk2])
                    cp("act", h2tok[i], pt2, [ptk2], ["h2tok%d" % i])
                    dma("sp", h2tok_d[b * SEQ + t * 128:b * SEQ + (t + 1) * 128, :], h2tok[i], "h2o%d" % i, ["h2tok%d" % i], [uk()])
                    ps, pk = nextps()
                    for kc in range(8):
                        mm(ps[:, 0:NE], h2f[:, kc, :], wr_f[:, kc, :], kc == 0, kc == 7, ["h2f", "wr_f"], [pk])
                    tt("dve", lg, ps[:, 0:NE], br_bc, ALU.add, [pk, "br_bc"], ["lg"])
                    P.op("dve", lambda e, o=mx8, i_=lg: e.max(out=o, in_=i_), ["lg"], ["mx8"])
                    ts("dve", msk, lg, mx8[:, 3:4], None, ALU.is_ge, None, ["lg", "mx8"], ["msk"])
                    ts("dve", nmx, mx8[:, 0:1], -1.0, None, ALU.mult, None, ["mx8"], ["nmx"])
                    act(ex, lg, AF.Exp, ["lg", "nmx"], ["ex"], bias=nmx)
                    tt("dve", ex, ex, msk, ALU.mult, ["ex", "msk"], ["ex"])
                    P.op("dve", lambda e, o=sm_, i_=ex: e.reduce_sum(out=o, in_=i_, axis=AX.X), ["ex"], ["sm"])
                    recip(sm_, sm_, ["sm"], ["sm"])
                    ts("dve", gates[b][:, t, :], ex, sm_, None, ALU.mult, None, ["ex", "sm"], ["gates%d" % b])

                p4_s1(0)
                for t in range(NT):
                    if t + 1 < NT:
                        p4_s1(t + 1)
                    p4_s2(t)
                if debug:
                    dma("sp", dbg_gates[b], gates[b].rearrange("p t e -> p (t e)"), "dbg5", ["gates%d" % b], ["dbg_gates"])
                P.barrier()
                A.reset(bm)

            A.reset(perm_mark)
            if stop_after == "mixer":
                return
            BS = MBS
            NBLK = MNBLK
            NSUB = BS // 128
            NTT = NB * NT
            rm = A.mark()
            dsel_i = A.f32(NTT * 4).bitcast(I32).rearrange("p (t k) -> p t k", k=4)
            gk = A.f32(NTT * 4).rearrange("p (t k) -> p t k", k=4)
            widx_i = A.f32(NBLK * 8).bitcast(I32).rearrange("p (b k) -> p b k", k=8)
            wgidx_i = A.f32(NBLK * 8).bitcast(I32).rearrange("p (b k) -> p b k", k=8)
            bidx_i = A.f32(NBLK).bitcast(I32)
            eb_i = A.f32(NBLK).bitcast(I32)
            idx_mark = A.mark()
            SU_bf = A.bf16(128)
            pk_f = A.f32(8)
            Mf = A.f32(NTT * NE)
            M_bf = A.bf16(NTT * NE)
            rk = A.f32(NTT * NE)
            cntb = A.f32(NTT * NE)
            offs = A.f32(NTT * NE)
            dest1 = A.f32(NTT * NE)
            count = A.f32(NE)
            nblk = A.f32(NE)
            pstart = A.f32(NE)
            pend = A.f32(NE)
            mx8 = A.f32(8)
            tmpe = A.f32(NE)
            dsel_f = A.f32(NTT * 4).rearrange("p (t k) -> p t k", k=4)
            ebf = A.f32(NBLK)
            empt = A.f32(NBLK)
            tmpb = A.f32(NBLK)
            widx_f = A.f32(NBLK * 8).rearrange("p (b k) -> p b k", k=8)
            v3e = lambda a: a.rearrange("p (t e) -> p t e", e=NE)
            gall = gates_all.rearrange("p t e -> p (t e)")
            dma("pool", SU_bf, SU_d, "r_su", [], ["SU_bf"])
            dma("sp", pk_f, PK_d, "r_pk", [], ["pk_f"])
            ts("dve", Mf, gall, 0.0, None, ALU.is_gt, None, ["gates0", "gates1"], ["Mf"])
            cp("dve", M_bf, Mf, ["Mf"], ["M_bf"])
            for hf in range(2):
                ps, pk = nextps()
                mm(ps, SU_bf, M_bf[:, hf * 512:(hf + 1) * 512], True, True, ["SU_bf", "M_bf"], [pk])
                cp("dve", rk[:, hf * 512:(hf + 1) * 512], ps, [pk], ["rk"])
                ps, pk = nextps()
                mm(ps, ones_bf, M_bf[:, hf * 512:(hf + 1) * 512], True, True, ["ones_bf", "M_bf"], [pk])
                cp("dve", cntb[:, hf * 512:(hf + 1) * 512], ps, [pk], ["cntb"])
            memset("dve", offs[:, 0:NE], 0.0, ["offs"])
            for T in range(1, NTT):
                tt("dve", offs[:, T * NE:(T + 1) * NE], offs[:, (T - 1) * NE:T * NE], cntb[:, (T - 1) * NE:T * NE], ALU.add, ["offs", "cntb"], ["offs"])
            tt("dve", count, offs[:, (NTT - 1) * NE:NTT * NE], cntb[:, (NTT - 1) * NE:NTT * NE], ALU.add, ["offs", "cntb"], ["count"])
            memset("dve", nblk, 0.0, ["nblk"])
            for k in range(-(-(NB * SEQ) // BS)):
                stt("dve", nblk, count, float(k * BS), nblk, ALU.is_gt, ALU.add, ["count", "nblk"], ["nblk"])
            ts("dve", nblk, nblk, float(BS), None, ALU.mult, None, ["nblk"], ["nblk"])
            memset("dve", pstart[:, 0:1], 0.0, ["pstart"])
            for e in range(1, NE):
                tt("dve", pstart[:, e:e + 1], pstart[:, e - 1:e], nblk[:, e - 1:e], ALU.add, ["pstart", "nblk"], ["pstart"])
            tt("dve", pend, pstart, nblk, ALU.add, ["pstart", "nblk"], ["pend"])
            tt("dve", v3e(offs), v3e(offs), pstart.unsqueeze(1).to_broadcast([128, NTT, NE]), ALU.add, ["offs", "pstart"], ["offs"])
            tt("dve", dest1, rk, offs, ALU.add, ["rk", "offs"], ["dest1"])
            stt("dve", dest1, dest1, 1.0, Mf, ALU.add, ALU.mult, ["dest1", "Mf"], ["dest1"])
            for T in range(NTT):
                P.op("dve", lambda e, o=mx8, i_=v3e(dest1)[:, T, :]: e.max(out=o, in_=i_), ["dest1"], ["rmx8"])
                ts("dve", dsel_f[:, T, :], mx8[:, 0:4], -1.0, None, ALU.add, None, ["rmx8"], ["dsel_f"])
                for k in range(4):
                    stt("dve", tmpe, v3e(dest1)[:, T, :], mx8[:, k:k + 1], v3e(gall)[:, T, :], ALU.is_equal, ALU.mult, ["dest1", "rmx8", "gates0", "gates1"], ["tmpe"])
                    P.op("dve", lambda e, o=gk[:, T, k:k + 1], i_=tmpe: e.reduce_sum(out=o, in_=i_, axis=AX.X), ["tmpe"], ["gk"])
            cp("dve", dsel_i.rearrange("p t k -> p (t k)"), dsel_f.rearrange("p t k -> p (t k)"), ["dsel_f"], ["dsel_i"])
            for blk in range(NBLK):
                ts("dve", tmpe, pend, float(blk * BS), None, ALU.is_le, None, ["pend"], ["tmpe"])
                P.op("dve", lambda e, o=ebf[:, blk:blk + 1], i_=tmpe: e.reduce_sum(out=o, in_=i_, axis=AX.X), ["tmpe"], ["ebf"])
                cp("dve", empt[:, blk:blk + 1], tmpe[:, NE - 1:NE], ["tmpe"], ["empt"])
            ts("dve", ebf, ebf, float(NE - 1), None, ALU.min, None, ["ebf"], ["ebf"])
            ts("dve", empt, empt, 1.0e6, None, ALU.mult, None, ["empt"], ["empt"])
            cp("dve", eb_i, ebf, ["ebf"], ["eb_i"])
            ts("dve", tmpb, ebf, 128.0, None, ALU.mult, None, ["ebf", "eb_i"], ["tmpb"])
            ts("dve", tmpb, tmpb, pk_f[:, 0:1], None, ALU.add, None, ["tmpb", "pk_f"], ["tmpb"])
            cp("dve", bidx_i, tmpb, ["tmpb"], ["bidx_i"])
            ts("dve", tmpb, ebf, 1024.0, None, ALU.mult, None, ["ebf", "bidx_i"], ["tmpb"])
            tt("dve", widx_f, tmpb.unsqueeze(2).to_broadcast([128, NBLK, 8]), pk_f.unsqueeze(1).to_broadcast([128, NBLK, 8]), ALU.add, ["tmpb", "pk_f"], ["widx_f"])
            cp("dve", widx_i.rearrange("p b k -> p (b k)"), widx_f.rearrange("p b k -> p (b k)"), ["widx_f"], ["widx_i"])
            tt("dve", widx_f, widx_f, empt.unsqueeze(2).to_broadcast([128, NBLK, 8]), ALU.add, ["widx_f", "empt", "widx_i"], ["widx_f"])
            cp("dve", wgidx_i.rearrange("p b k -> p (b k)"), widx_f.rearrange("p b k -> p (b k)"), ["widx_f"], ["wgidx_i"])
            if debug:
                dma("sp", dbg_route[:, 0:NTT * 4], dsel_f.rearrange("p t k -> p (t k)"), "dbgr", ["dsel_f"], ["dbg_route"])
                dma("sp", dbg_route[:, NTT * 4:NTT * 8], gk.rearrange("p t k -> p (t k)"), "dbgr", ["gk"], ["dbg_route"])
                dma("sp", dbg_route[:, NTT * 8:NTT * 8 + NBLK], ebf, "dbgr", ["ebf"], ["dbg_route"])
            P.barrier()
            A.reset(idx_mark)
            chk("route")
            dm = A.mark()
            hx_ = [A.bf16(D) for _ in range(4)]
            for T in range(NTT):
                i = T % 4
                dma("sp", hx_[i], h2tok_d[T * 128:(T + 1) * 128, :], "dl%d" % i, ["h2tok_d"], ["hx%d" % i])
                for k in range(4):
                    P.dma("pool", lambda e, o=xsorted_d[:, :], ix=dsel_i[:, T, k:k + 1], src=hx_[i]: e.indirect_dma_start(
                        out=o, out_offset=bass.IndirectOffsetOnAxis(ap=ix, axis=0), in_=src, in_offset=None),
                        "ds%d" % i, ["hx%d" % i, "dsel_i", "xsorted_d"], [uk()])
            P.barrier()
            A.reset(dm)
            chk("dispatch")
            em = A.mark()
            Wgu = [A.bf16(8 * 2 * D).rearrange("p (k n) -> p k n", k=8) for _ in range(2)]
            Wd = [A.bf16(8 * D).rearrange("p (k n) -> p k n", k=8) for _ in range(2)]
            bgu = [A.f32(16) for _ in range(2)]
            bdr = [A.bf16(D) for _ in range(2)]
            xblk = [A.bf16(NSUB * D).rearrange("p (s n) -> p s n", s=NSUB) for _ in range(2)]
            xT = [A.bf16(8 * BS).rearrange("p (k t) -> p k t", k=8) for _ in range(2)]
            actT = [A.bf16(8 * BS).rearrange("p (k t) -> p k t", k=8) for _ in range(2)]
            yt = [A.f32(D) for _ in range(2)]
            glu = [A.f32(512) for _ in range(2)]
            sig = [A.f32(512) for _ in range(2)]
            l1 = [A.f32(512) for _ in range(2)]
            tq = [A.f32(512) for _ in range(2)]
            wgu_rows = w_gu_d.rearrange("e k n -> (e k) n")
            bregs = {}

            def breg(e, val):
                if val not in bregs:
                    r = e.alloc_register("bnd%d" % val)
                    e.reg_mov(r, val)
                    bregs[val] = r
                return bregs[val]
            wd_rows = w_down_d.rearrange("e k n -> (e k) n")
            uc = 0
            yc = 0
            for blk in range(NBLK):
                s = blk % 2
                for kc in range(8):
                    P.dma("pool", lambda e, o=Wgu[s][:, kc, :], ix=wgidx_i[:, blk, kc:kc + 1]: e.indirect_dma_start(
                        out=o, out_offset=None, in_=wgu_rows[:, :], in_offset=bass.IndirectOffsetOnAxis(ap=ix, axis=0),
                        bounds_check=breg(e, NE * D - 1), oob_is_err=False),
                        "Wgu%d" % s, ["wgidx_i"], ["Wgu%d_%d" % (s, kc)])
                P.dma("pool", lambda e, o=bgu[s], ix=bidx_i[:, blk:blk + 1]: e.indirect_dma_start(
                    out=o, out_offset=None, in_=bguFM_d[:, :], in_offset=bass.IndirectOffsetOnAxis(ap=ix, axis=0)),
                    "bgu%d" % s, ["bidx_i"], ["bgu%d" % s])
                ts("dve", bgu[s][:, 8:16], bgu[s][:, 8:16], 1.0, None, ALU.add, None, ["bgu%d" % s], ["bgu%d" % s])
                P.dma("pool", lambda e, o=bdr[s][0:2, :], ix=eb_i[0:2, blk:blk + 1]: e.indirect_dma_start(
                    out=o, out_offset=None, in_=bd_row_d[:, :], in_offset=bass.IndirectOffsetOnAxis(ap=ix, axis=0)),
                    "bdr%d" % s, ["eb_i"], ["bdr%d" % s])
                for kc in range(8):
                    P.dma("pool", lambda e, o=Wd[s][:, kc, :], ix=wgidx_i[:, blk, kc:kc + 1]: e.indirect_dma_start(
                        out=o, out_offset=None, in_=wd_rows[:, :], in_offset=bass.IndirectOffsetOnAxis(ap=ix, axis=0),
                        bounds_check=breg(e, NE * D - 1), oob_is_err=False),
                        "Wd%d" % s, ["wgidx_i"], ["Wd%d_%d" % (s, kc)])
                if blk == 0:
                    dma("sp", xblk[0], xsorted_d[0:BS, :].rearrange("(s p) n -> p s n", p=128), "xb0", ["xsorted_d"], ["xblk0"])
                for sub in range(NSUB):
                    pt, ptk = nextpt()
                    ptv = pt.rearrange("p (k t) -> p k t", k=8)
                    for kc in range(8):
                        tr(ptv[:, kc, :], xblk[s][:, sub, kc * 128:(kc + 1) * 128], IDB, ["xblk%d" % s, "IDB"], [ptk])
                    cp("act" if sub % 2 == 0 else "dve", xT[s][:, :, sub * 128:(sub + 1) * 128], ptv, [ptk], ["xT%d" % s])
                if blk + 1 < NBLK:
                    dma("sp", xblk[1 - s], xsorted_d[(blk + 1) * BS:(blk + 2) * BS, :].rearrange("(s p) n -> p s n", p=128), "xb%d" % (1 - s),
                        ["xsorted_d"], ["xblk%d" % (1 - s)])
                for j in range(8):
                    i = uc % 2
                    uc += 1
                    psg, pkg = nextps()
                    for kc in range(8):
                        mm(psg[:, 0:BS], Wgu[s][:, kc, j * 128:(j + 1) * 128], xT[s][:, kc, :], kc == 0, kc == 7, ["Wgu%d_%d" % (s, kc), "xT%d" % s], [pkg])
                    psl, pkl = nextps()
                    for kc in range(8):
                        mm(psl[:, 0:BS], Wgu[s][:, kc, D + j * 128:D + (j + 1) * 128], xT[s][:, kc, :], kc == 0, kc == 7, ["Wgu%d_%d" % (s, kc), "xT%d" % s], [pkl])
                    ts("dve", glu[i][:, 0:BS], psg[:, 0:BS], bgu[s][:, j:j + 1], 7.0, ALU.add, ALU.min, [pkg, "bgu%d" % s], ["glu%d" % i])
                    act(sig[i][:, 0:BS], glu[i][:, 0:BS], AF.Sigmoid, ["glu%d" % i], ["sig%d" % i], scale=1.702)
                    ts("dve", l1[i][:, 0:BS], psl[:, 0:BS], bgu[s][:, 8 + j:9 + j], -6.0, ALU.add, ALU.max, [pkl, "bgu%d" % s], ["l1%d" % i])
                    tt("dve", tq[i][:, 0:BS], glu[i][:, 0:BS], sig[i][:, 0:BS], ALU.mult, ["glu%d" % i, "sig%d" % i], ["tq%d" % i])
                    stt("dve", actT[s][:, j, :], l1[i][:, 0:BS], 8.0, tq[i][:, 0:BS], ALU.min, ALU.mult, ["l1%d" % i, "tq%d" % i], ["actT%d" % s])
                for sub in range(NSUB):
                    yi = yc % 2
                    yc += 1
                    for hf in range(2):
                        ps, pk = nextps()
                        mm(ps, ones_bf[0:1, 0:128], bdr[s][0:1, hf * 512:(hf + 1) * 512], True, False, ["ones_bf", "bdr%d" % s], [pk])
                        for j in range(8):
                            mm(ps, actT[s][:, j, sub * 128:(sub + 1) * 128], Wd[s][:, j, hf * 512:(hf + 1) * 512], False, j == 7, ["actT%d" % s, "Wd%d_%d" % (s, j)], [pk])
                        cp("act", yt[yi][:, hf * 512:(hf + 1) * 512], ps, [pk], ["yt%d" % yi])
                    r0 = blk * BS + sub * 128
                    dma("sp", ysorted_d[r0:r0 + 128, :], yt[yi], "yo%d" % yi, ["yt%d" % yi], [uk()])
            P.barrier()
            A.reset(em)
            chk("experts")
            yk = [[A.f32(D) for _ in range(4)] for _ in range(3)]
            acc = [A.f32(D) for _ in range(3)]
            xt = [A.f32(D) for _ in range(3)]
            junk = A.f32(D)
            ss = A.f32(2)
            rs = A.f32(2)
            for T in range(NTT):
                b, t = T // NT, T % NT
                i = T % 3
                for k in range(4):
                    P.dma("pool", lambda e, o=yk[i][k], ix=dsel_i[:, T, k:k + 1]: e.indirect_dma_start(
                        out=o, out_offset=None, in_=ysorted_d[:, :], in_offset=bass.IndirectOffsetOnAxis(ap=ix, axis=0)),
                        "yk%d_%d" % (i, k), ["dsel_i", "ysorted_d"], ["yk%d_%d" % (i, k)])
                dma("sp", xt[i], x1_d[b, t * 128:(t + 1) * 128, :], "fx%d" % i, ["x1_d"], ["fxt%d" % i])
                ts("dve", acc[i], yk[i][0], gk[:, T, 0:1], None, ALU.mult, None, ["yk%d_0" % i, "gk"], ["acc%d" % i])
                for k in range(1, 4):
                    stt("dve", acc[i], yk[i][k], gk[:, T, k:k + 1], acc[i], ALU.mult, ALU.add, ["yk%d_%d" % (i, k), "gk", "acc%d" % i], ["acc%d" % i])
                tt("dve", acc[i], acc[i], g2_bc[b], ALU.mult, ["acc%d" % i, "g2_bc%d" % b], ["acc%d" % i])
                tt("dve", acc[i], acc[i], xt[i], ALU.add, ["acc%d" % i, "fxt%d" % i], ["acc%d" % i])
                memset("dve", ss[:, 0:1], 0.0, ["fss"])
                act(junk, acc[i], AF.Square, ["acc%d" % i], ["fjunk", "fss"], accum=ss[:, 0:1])
                rstd_from_ss(rs[:, 0:1], ss[:, 0:1], D, "f")
                stt("dve", acc[i], acc[i], rs[:, 0:1], fg_bc, ALU.mult, ALU.mult, ["acc%d" % i, "frs", "fg_bc"], ["acc%d" % i])
                dma("sp", out_d[b, t * 128:(t + 1) * 128, :], acc[i], "fo%d" % i, ["acc%d" % i], [uk()], output=True)
            P.barrier()
        try:
            _body()
        except _Stop:
            pass
        P.finalize_and_emit(st)
        build.stats = P.stats
    return nc


def _consts():
    k = np.arange(128)[:, None]
    i = np.arange(128)[None, :]
    U = (k <= i).astype(np.float32)
    L = (k >= i).astype(np.float32)
    NEGF = np.where(i < k, -30000.0, 0.0).astype(np.float32)
    NEGB = np.where(i > k, -30000.0, 0.0).astype(np.float32)
    IDF = np.eye(128, dtype=np.float32)
    SU = (k < i).astype(np.float32)
    PK = (np.arange(128)[:, None] + 128 * np.arange(8)[None, :]).astype(np.float32)
    return dict(cU=U, cL=L, cNEGF=NEGF, cNEGB=NEGB, cIDF=IDF, cSU=SU, cPK=PK)


def fmaj(v, nch):
    return np.ascontiguousarray(np.asarray(v, np.float32).reshape(nch, 128).T)


def make_in_maps(inputs, cores):
    f = lambda a: np.ascontiguousarray(np.asarray(a, dtype=np.float32))
    x, c, ctx, c_ctx = f(inputs["x"]), f(inputs["c"]), f(inputs["ctx"]), f(inputs["c_ctx"])
    shared = dict(
        w_mod=f(inputs["w_mod"][0]),
        b_modT=fmaj(inputs["b_mod"][0], 48),
        b_mod_row=f(inputs["b_mod"][0]).reshape(1, -1),
        n1gT=fmaj(inputs["norm1_g"][0], 8),
        n2gT=fmaj(inputs["norm2_g"][0], 8),
        w_in=f(inputs["w_in"][0]),
        convw=f(inputs["ssd_conv_w"][0]),
        convb_row=f(inputs["ssd_conv_b"][0]).reshape(1, -1),
        convbT=fmaj(inputs["ssd_conv_b"][0], 12),
        dtb_row=f(inputs["ssd_dt_bias"][0]).reshape(1, 32),
        alog_row=f(inputs["ssd_a_log"][0]).reshape(1, 32),
        d_row=f(inputs["ssd_d"][0]).reshape(1, 16),
        gssd_row=f(inputs["ssd_norm_g"][0]).reshape(1, -1),
        scwT=np.ascontiguousarray(f(inputs["sc_conv_w"][0]).reshape(3, 8, 128).transpose(2, 1, 0)),
        w_out=f(inputs["w_out"][0]),
        w_router=f(inputs["w_router"][0]),
        br_row=f(inputs["b_router"][0]).reshape(1, -1),
        w_gu=f(inputs["w_gate_up"][0]),
        bguT=np.ascontiguousarray(f(inputs["b_gate_up"][0]).reshape(NE, 16, 128).transpose(2, 0, 1)),
        w_down=f(inputs["w_down"][0]),
        bguFM=np.ascontiguousarray(f(inputs["b_gate_up"][0]).reshape(NE, 16, 128).transpose(0, 2, 1).reshape(NE * 128, 16)),
        bd_row=f(inputs["b_down"][0]),
        fg_row=f(inputs["final_g"]).reshape(1, -1),
    )
    shared.update(_consts())
    maps = []
    for ci in cores:
        vecs = np.stack([c[2 * ci], c[2 * ci + 1], c_ctx], axis=1)
        cT = np.ascontiguousarray(vecs.reshape(8, 128, 3).transpose(1, 0, 2))
        m = dict(shared)
        m["x"] = np.ascontiguousarray(x[2 * ci:2 * ci + 2])
        m["ctx"] = np.ascontiguousarray(ctx[2 * ci:2 * ci + 2])
        m["cT"] = cT
        maps.append(m)
    return maps


def kernel(**inputs):
    nc = build(debug=False)
    maps = make_in_maps(inputs, list(range(8)))
    res = run_bass_kernel_spmd(nc, maps, core_ids=list(range(8)))
    return np.concatenate([np.asarray(r["out"], dtype=np.float32) for r in res.results], axis=0)
```

```python
import numpy as np
from contextlib import ExitStack
import concourse.bass as bass
import concourse.mybir as mybir
from concourse.bass_utils import run_bass_kernel_spmd

F32 = mybir.dt.float32
BF16 = mybir.dt.bfloat16
I32 = mybir.dt.int32
ALU = mybir.AluOpType
AF = mybir.ActivationFunctionType
AX = mybir.AxisListType

ENGS = ("pe", "dve", "act", "pool", "sp")

D = 1024
SEQ = 2048
CTX = 256
NB = 2
NT = SEQ // 128
IN_W = 5664
X0, B0, C0, DT0, SC0 = 1024, 2048, 2304, 2560, 2592
NE = 32
EPS = 1e-6
MBS = 384
MNBLK = -(-(NB * SEQ * 4 + NE * (MBS - 1)) // MBS)
HP = 16


class Op:
    __slots__ = ("eng", "fn", "deps", "needs_inc", "val", "is_dma", "dsem", "waits")

    def __init__(self, eng, fn, is_dma=False, dsem=None):
        self.eng = eng
        self.fn = fn
        self.deps = []
        self.needs_inc = False
        self.val = None
        self.is_dma = is_dma
        self.dsem = dsem
        self.waits = None


class Prog:
    def __init__(self, nc):
        self.nc = nc
        self.ops = {e: [] for e in ENGS}
        self.res = {}
        self.dma_counts = {}
        self.last_dma = {}
        self.out_dma_ops = []
        self.dmap = {}
        self.free_phys = []
        self.nphys = 0
        self.bg = set()
        self.bgkeys = {}
        self.free_by_cls = {}
        self.phys_cls = {}

    def _add(self, op, reads, writes):
        deps = []
        res = self.res
        for k in reads:
            st = res.get(k)
            if st is None:
                st = res[k] = [None, {}]
            if st[0] is not None:
                deps.append(st[0])
        for k in writes:
            st = res.get(k)
            if st is None:
                st = res[k] = [None, {}]
            if st[0] is not None:
                deps.append(st[0])
            deps.extend(st[1].values())
        rk = ("d", op.dsem) if op.is_dma else op.eng
        for k in reads:
            res[k][1][rk] = op
        for k in writes:
            st = res[k]
            st[0] = op
            st[1] = {}
        seen = set()
        for d in deps:
            if d is op or id(d) in seen:
                continue
            seen.add(id(d))
            if (not d.is_dma) and (not op.is_dma) and d.eng == op.eng and op.eng == "pe":
                continue
            op.deps.append(d)
            d.needs_inc = True
        self.ops[op.eng].append(op)
        return op

    def op(self, eng, fn, reads=(), writes=()):
        return self._add(Op(eng, fn), reads, writes)

    def dma(self, queue, fn, dsem, reads=(), writes=(), is_output=False, background=False):
        if background and dsem not in self.dmap:
            self.dmap[dsem] = self.nphys
            self.phys_cls[self.nphys] = "bg"
            self.bg.add(self.nphys)
            self.bgkeys[dsem] = self.nphys
            self.nphys += 1
        if dsem not in self.dmap:
            cls = "sw" if queue == "pool" else "hw"
            fl = self.free_by_cls.setdefault(cls, [])
            if fl:
                self.dmap[dsem] = fl.pop()
            else:
                self.dmap[dsem] = self.nphys
                self.phys_cls[self.nphys] = cls
                self.nphys += 1
        dsem = self.dmap[dsem]
        op = Op(queue, fn, is_dma=True, dsem=dsem)
        self.dma_counts[dsem] = self.dma_counts.get(dsem, 0) + 1
        op.val = 16 * self.dma_counts[dsem]
        op.needs_inc = True
        self._add(op, reads, writes)
        self.last_dma[dsem] = op
        if is_output:
            self.out_dma_ops.append(op)
        return op

    def barrier(self):
        lasts = []
        for e in ENGS:
            for op in reversed(self.ops[e]):
                if op.fn is not None and not op.is_dma:
                    lasts.append(op)
                    break
        dl = [op_ for k_, op_ in self.last_dma.items() if k_ not in self.bg]
        for e in ENGS:
            b = Op(e, None)
            b.deps = [d for d in lasts if not (d.eng == e and e == "pe")] + dl
            for d in b.deps:
                d.needs_inc = True
            self.ops[e].append(b)
        for v_ in self.dmap.values():
            if v_ not in self.bg:
                self.free_by_cls.setdefault(self.phys_cls[v_], []).append(v_)
        self.dmap = dict(self.bgkeys)

    def finalize_and_emit(self, stack):
        nc = self.nc
        fin = Op("sp", None)
        fin.deps = list(self.out_dma_ops)
        self.ops["sp"].append(fin)
        for e in ENGS:
            c = 0
            for op in self.ops[e]:
                if op.is_dma:
                    continue
                if op.needs_inc:
                    c += 1
                    op.val = c
        esem = {e: stack.enter_context(nc.semaphore("s_" + e)) for e in ENGS}
        dsems = {k: stack.enter_context(nc.semaphore("d_" + str(k))) for k in self.dma_counts}
        nwaits = 0
        for e in ENGS:
            seen = {}
            for op in self.ops[e]:
                w = {}
                for d in op.deps:
                    s = ("d", d.dsem) if d.is_dma else ("e", d.eng)
                    if seen.get(s, 0) >= d.val:
                        continue
                    if w.get(s, 0) < d.val:
                        w[s] = d.val
                for s, v in w.items():
                    seen[s] = v
                op.waits = [((dsems[s[1]] if s[0] == "d" else esem[s[1]]), v) for s, v in w.items()]
                nwaits += len(op.waits)
        self.stats = {e: len(self.ops[e]) for e in ENGS}
        self.stats["waits"] = nwaits
        self.stats["dsems"] = len(dsems)
        block = stack.enter_context(nc.Block())
        ops = self.ops

        def replay(engname, eng):
            for op in ops[engname]:
                for (s, v) in op.waits:
                    eng.wait_ge(s, v)
                if op.fn is None:
                    continue
                ins = op.fn(eng)
                if op.is_dma:
                    ins.then_inc(dsems[op.dsem], 16)
                elif op.needs_inc:
                    ins.then_inc(esem[engname], 1)

        @block.tensor
        def _(eng):
            replay("pe", eng)

        @block.vector
        def _(eng):
            replay("dve", eng)

        @block.scalar
        def _(eng):
            replay("act", eng)

        @block.gpsimd
        def _(eng):
            replay("pool", eng)

        @block.sync
        def _(eng):
            replay("sp", eng)


class _Stop(Exception):
    pass


class Arena:
    def __init__(self, ap, n):
        self.ap, self.n, self.off = ap, n, 0

    def f32(self, n):
        assert self.off + n <= self.n, ("arena overflow", self.off, n, self.n)
        v = self.ap[:, self.off:self.off + n]
        self.off += n
        return v

    def bf16(self, n):
        m = (n + 1) // 2
        assert self.off + m <= self.n, ("arena overflow", self.off, m, self.n)
        v = self.ap[:, self.off:self.off + m].bitcast(BF16)
        self.off += m
        return v

    def mark(self):
        return self.off

    def reset(self, m):
        self.off = m


def build(debug=False, stop_after=None):
    nc = bass.Bass("TRN2", target_bir_lowering=False)

    def din(name, shape):
        return nc.dram_tensor(name, shape, F32, kind="ExternalInput").ap()

    scr_kind = "ExternalOutput" if debug else "Internal"

    def dscr(name, shape, dt):
        return nc.dram_tensor(name, shape, dt, kind=scr_kind).ap()

    x_d = din("x", [NB, SEQ, D])
    ctx_d = din("ctx", [NB, CTX, D])
    cT_d = din("cT", [128, 8, 3])
    w_mod_d = din("w_mod", [D, 6 * D])
    b_modT_d = din("b_modT", [128, 48])
    b_mod_row_d = din("b_mod_row", [1, 6 * D])
    n1gT_d = din("n1gT", [128, 8])
    n2gT_d = din("n2gT", [128, 8])
    w_in_d = din("w_in", [D, IN_W])
    convw_d = din("convw", [3, 1536])
    convb_row_d = din("convb_row", [1, 1536])
    convbT_d = din("convbT", [128, 12])
    dtb_row_d = din("dtb_row", [1, 32])
    alog_row_d = din("alog_row", [1, 32])
    d_row_d = din("d_row", [1, 16])
    gssd_row_d = din("gssd_row", [1, D])
    scwT_d = din("scwT", [128, 8, 3])
    w_out_d = din("w_out", [2 * D, D])
    w_router_d = din("w_router", [D, NE])
    br_row_d = din("br_row", [1, NE])
    w_gu_d = din("w_gu", [NE, D, 2 * D])
    bguT_d = din("bguT", [128, NE, 16])
    w_down_d = din("w_down", [NE, D, D])
    bd_row_d = din("bd_row", [NE, D])
    fg_row_d = din("fg_row", [1, D])
    U_d = din("cU", [128, 128])
    L_d = din("cL", [128, 128])
    NEGF_d = din("cNEGF", [128, 128])
    NEGB_d = din("cNEGB", [128, 128])
    IDF_d = din("cIDF", [128, 128])
    out_d = nc.dram_tensor("out", [NB, SEQ, D], F32, kind="ExternalOutput").ap()

    sz_d = dscr("sz_s", [NB, SEQ, D], BF16)
    yscT_d = dscr("yscT_s", [NB, 8, 128, SEQ], BF16)
    yssdT_d = dscr("yssdT_s", [NB, 8, 128, SEQ], BF16)
    x1_d = dscr("x1_s", [NB, SEQ, D], F32)
    h2tok_d = dscr("h2tok_s", [NB * SEQ, D], BF16)
    NROWS = MNBLK * MBS
    xsorted_d = dscr("xsorted_s", [NROWS, D], BF16)
    ysorted_d = dscr("ysorted_s", [NROWS, D], F32)
    bguFM_d = din("bguFM", [NE * 128, 16])
    SU_d = din("cSU", [128, 128])
    PK_d = din("cPK", [128, 8])
    if debug:
        dbg_route = dscr("dbg_route", [128, NB * NT * 8 + 128], F32)
    if debug:
        dbg_xs = dscr("dbg_xs", [NB, 128, NT * D], BF16)
        dbg_dt = dscr("dbg_dt", [NB, 128, NT * 32], F32)
        dbg_h0 = dscr("dbg_h0", [NB, 2, 128, D], F32)
        dbg_gates = dscr("dbg_gates", [NB, 128, NT * NE], F32)
        dbg_hT = dscr("dbg_hT", [NB, 128, 8 * (SEQ + 2 * HP)], BF16)
        dbg_BT = dscr("dbg_BT", [NB, 2, 128, 2 * SEQ], BF16)

    w_in_v = w_in_d.rearrange("(kc p) n -> p kc n", p=128)
    w_mod_v = w_mod_d.rearrange("(kc p) n -> p kc n", p=128)
    w_out_v = w_out_d.rearrange("(kc p) n -> p kc n", p=128)
    w_router_v = w_router_d.rearrange("(kc p) n -> p kc n", p=128)

    with ExitStack() as st:
        AR = 52992
        arena_t = st.enter_context(nc.sbuf_tensor("arena", [128, AR], F32))
        A = Arena(arena_t[:, :], AR)
        psf = [st.enter_context(nc.psum_tensor("psf%d" % i, [128, 512], F32)) for i in range(6)]
        ptb = [st.enter_context(nc.psum_tensor("ptb%d" % i, [128, 1024], BF16)) for i in range(2)]
        P = Prog(nc)
        psc = [0]
        psn = [5]
        ukc = [0]

        def uk():
            ukc[0] += 1
            return "uk%d" % ukc[0]

        def nextps():
            i = psc[0] % psn[0]
            psc[0] += 1
            return psf[i][:, :], "ps%d" % i

        ptc = [0]

        def nextpt():
            i = ptc[0] % 2
            ptc[0] += 1
            return ptb[i][:, :], "pt%d" % i

        def mm(out, lhsT, rhs, start, stop, R, W):
            P.op("pe", lambda e: e.matmul(out, lhsT=lhsT, rhs=rhs, start=start, stop=stop), R, W)

        def tr(out, in_, ident, R, W):
            P.op("pe", lambda e: e.transpose(out, in_, ident), R, W)

        def tt(eng, out, in0, in1, op, R, W):
            P.op(eng, lambda e: e.tensor_tensor(out=out, in0=in0, in1=in1, op=op), R, W)

        def ts(eng, out, in0, s1, s2, op0, op1, R, W):
            if op1 is None:
                P.op(eng, lambda e: e.tensor_scalar(out=out, in0=in0, scalar1=s1, scalar2=None, op0=op0), R, W)
            else:
                P.op(eng, lambda e: e.tensor_scalar(out=out, in0=in0, scalar1=s1, scalar2=s2, op0=op0, op1=op1), R, W)

        def stt(eng, out, in0, scalar, in1, op0, op1, R, W):
            P.op(eng, lambda e: e.scalar_tensor_tensor(out=out, in0=in0, scalar=scalar, in1=in1, op0=op0, op1=op1), R, W)

        def act(out, in_, func, R, W, bias=None, scale=None, accum=None):
            kw = {}
            if bias is not None:
                kw["bias"] = bias
            if scale is not None:
                kw["scale"] = scale
            if accum is not None:
                kw["accum_out"] = accum
            P.op("act", lambda e: e.activation(out=out, in_=in_, func=func, **kw), R, W)

        def cp(eng, out, in_, R, W):
            if eng == "act":
                P.op("act", lambda e: e.copy(out=out, in_=in_), R, W)
            else:
                P.op(eng, lambda e: e.tensor_copy(out=out, in_=in_), R, W)

        def memset(eng, ap, val, W):
            P.op(eng, lambda e: e.memset(ap, val), (), W)

        def recip(out, in_, R, W):
            P.op("dve", lambda e: e.reciprocal(out=out, in_=in_), R, W)

        def dma(q, out, in_, sem, R, W, output=False, background=False):
            P.dma(q, lambda e: e.dma_start(out=out, in_=in_), sem, R, W, is_output=output, background=background)

        def rstd_from_ss(rstd, ss, n, key):
            ts("dve", rstd, ss, 1.0 / n, EPS, ALU.mult, ALU.add, [key + "ss"], [key + "rs"])
            act(rstd, rstd, AF.Sqrt, [key + "rs"], [key + "rs"])
            recip(rstd, rstd, [key + "rs"], [key + "rs"])

        ones_bf = A.bf16(128)
        bguT = A.f32(NE * 16).rearrange("p (e c) -> p e c", e=NE)
        gates_all = A.f32(NB * NT * NE).rearrange("p (t e) -> p t e", e=NE)
        gates = [gates_all[:, b_ * NT:(b_ + 1) * NT, :] for b_ in range(NB)]
        g2_bc = [A.f32(D) for _ in range(NB)]
        fg_bc = A.f32(D)
        IDB = A.bf16(128)
        G2 = A.f32(NB * 8).rearrange("p (v k) -> p v k", v=NB)
        S2 = A.f32(NB * 8).rearrange("p (v k) -> p v k", v=NB)
        perm_mark = A.mark()

        U = A.f32(128)
        L = A.f32(128)
        NEGF = A.f32(128)
        NEGB = A.f32(128)
        IDF = A.f32(128)
        ONESF = A.f32(128)
        G1 = A.f32(3 * 8).rearrange("p (v k) -> p v k", v=3)
        S1 = A.f32(3 * 8).rearrange("p (v k) -> p v k", v=3)
        dtb_bc = A.f32(32)
        A_bc = A.f32(32)
        D_bc = A.f32(16)
        convbT = A.f32(12)
        scwT = A.f32(24).rearrange("p (j k) -> p j k", j=8)
        br_bc = A.f32(NE)
        wr_f = A.f32(8 * NE).rearrange("p (k n) -> p k n", k=8)
        gssd_bc = A.f32(D)
        g1_bc = [A.f32(D) for _ in range(NB)]
        H0 = [A.f32(D) for _ in range(2)]
        convb_bf = A.bf16(1536)
        zt = A.bf16(2048)
        mixc_mark = A.mark()

        def chk(name):
            if stop_after == name:
                raise _Stop()

        def _body():
            for (t, src, k) in [(U, U_d, "U"), (L, L_d, "L"), (NEGF, NEGF_d, "NEGF"), (NEGB, NEGB_d, "NEGB"), (IDF, IDF_d, "IDF")]:
                dma("sp", t, src, "c_" + k, [], [k])
            dma("pool", IDB, IDF_d, "c_IDB", [], ["IDB"])
            memset("dve", ONESF, 1.0, ["ONESF"])
            memset("dve", ones_bf, 1.0, ["ones_bf"])
            dma("sp", bguT, bguT_d, "c_bgu", [], ["bguT"])
            ts("dve", bguT[:, :, 8:16], bguT[:, :, 8:16], 1.0, None, ALU.add, None, ["bguT"], ["bguT"])
            dma("sp", fg_bc, fg_row_d.partition_broadcast(128), "c_fg", [], ["fg_bc"])
            dma("sp", gssd_bc, gssd_row_d.partition_broadcast(128), "c_gssd", [], ["gssd_bc"])
            dma("sp", dtb_bc, dtb_row_d.partition_broadcast(128), "c_dtb", [], ["dtb_bc"])
            dma("sp", A_bc, alog_row_d.partition_broadcast(128), "c_alog", [], ["A_bc"])
            act(A_bc, A_bc, AF.Exp, ["A_bc"], ["A_bc"])
            ts("dve", A_bc, A_bc, -1.0, None, ALU.mult, None, ["A_bc"], ["A_bc"])
            dma("sp", D_bc, d_row_d.partition_broadcast(128), "c_d", [], ["D_bc"])
            dma("sp", convbT, convbT_d, "c_cbT", [], ["convbT"])
            dma("sp", scwT, scwT_d, "c_scw", [], ["scwT"])
            dma("sp", br_bc, br_row_d.partition_broadcast(128), "c_br", [], ["br_bc"])
            dma("sp", wr_f, w_router_v, "c_wr", [], ["wr_f"])
            dma("pool", convb_bf[0:1, :], convb_row_d, "c_cbb", [], ["convb_bf"])

            m0 = A.mark()
            cT_f = A.f32(24).rearrange("p (k v) -> p k v", k=8)
            scT_bf = A.bf16(24).rearrange("p (k v) -> p k v", k=8)
            scB = [A.bf16(8 * 128).rearrange("p (k m) -> p k m", k=8) for _ in range(NB)]
            b_modT = A.f32(48)
            n1gT = A.f32(8)
            n2gT = A.f32(8)
            modT = A.f32(48 * 3).rearrange("p (o v) -> p o v", o=48)
            bmod_bc = [A.f32(D) for _ in range(2)]
            wm = [A.bf16(8 * 128).rearrange("p (k n) -> p k n", k=8) for _ in range(3)]
            dma("sp", cT_f, cT_d, "m_cT", [], ["cT_f"])
            dma("sp", b_modT, b_modT_d, "m_bmT", [], ["b_modT"])
            dma("sp", n1gT, n1gT_d, "m_n1", [], ["n1gT"])
            dma("sp", n2gT, n2gT_d, "m_n2", [], ["n2gT"])
            dma("sp", bmod_bc[0], b_mod_row_d[:, 2 * D:3 * D].partition_broadcast(128), "m_bb0", [], ["bmod_bc0"])
            dma("sp", bmod_bc[1], b_mod_row_d[:, 5 * D:6 * D].partition_broadcast(128), "m_bb1", [], ["bmod_bc1"])
            act(scT_bf, cT_f, AF.Silu, ["cT_f"], ["scT_bf"])
            for v in range(NB):
                cp("dve", scB[v], scT_bf[:, :, v:v + 1].to_broadcast([128, 8, 128]), ["scT_bf"], ["scB%d" % v])
            for oc in range(48):
                mod, cc = oc // 8, oc % 8
                s = oc % 3
                dma("pool", wm[s], w_mod_v[:, :, oc * 128:(oc + 1) * 128], "m_wm%d" % s, [], ["wm%d" % s])
                if mod in (0, 1, 3, 4):
                    ps, pk = nextps()
                    for kc in range(8):
                        mm(ps[:, 0:3], wm[s][:, kc, :], scT_bf[:, kc, :], kc == 0, kc == 7, ["wm%d" % s, "scT_bf"], [pk])
                    ts("dve", modT[:, oc, :], ps[:, 0:3], b_modT[:, oc:oc + 1], None, ALU.add, None, [pk, "b_modT"], ["modT"])
                else:
                    gi = 0 if mod == 2 else 1
                    for v in range(NB):
                        ps, pk = nextps()
                        for kc in range(8):
                            mm(ps[:, 0:128], scB[v][:, kc, :], wm[s][:, kc, :], kc == 0, kc == 7, ["wm%d" % s, "scB%d" % v], [pk])
                        dst = (g1_bc if mod == 2 else g2_bc)[v]
                        tt("dve", dst[:, cc * 128:(cc + 1) * 128], ps[:, 0:128], bmod_bc[gi][:, cc * 128:(cc + 1) * 128], ALU.add,
                           [pk, "bmod_bc%d" % gi], ["g%d_bc%d" % (1 if mod == 2 else 2, v)])
            for v in range(3):
                stt("dve", G1[:, v, :], modT[:, 8:16, v], 1.0, n1gT, ALU.add, ALU.mult, ["modT", "n1gT"], ["G1"])
                cp("dve", S1[:, v, :], modT[:, 0:8, v], ["modT"], ["S1"])
            for v in range(NB):
                stt("dve", G2[:, v, :], modT[:, 32:40, v], 1.0, n2gT, ALU.add, ALU.mult, ["modT", "n2gT"], ["G2"])
                cp("dve", S2[:, v, :], modT[:, 24:32, v], ["modT"], ["S2"])
            P.barrier()
            A.reset(m0)
            chk("phase0")

            def norm_mod_T(src_rows, ntiles, Gv, Sv, hT, hkey, tag):
                mk = A.mark()
                xt = [A.f32(D) for _ in range(2)]
                xn = [A.bf16(D) for _ in range(2)]
                junk = A.f32(D)
                tmp = [A.f32(D).rearrange("p (k t) -> p k t", k=8) for _ in range(2)]
                ss = A.f32(4)
                rs = A.f32(4)
                memset("dve", hT[:, :, 0:HP], 0.0, [hkey])
                memset("dve", hT[:, :, ntiles * 128 + HP:ntiles * 128 + 2 * HP], 0.0, [hkey])
                for t in range(ntiles):
                    i = t % 2
                    k = "%s%d" % (tag, i)
                    dma("sp", xt[i], src_rows(t), "nm_x%d" % i, [], [k + "xt"])
                    memset("dve", ss[:, i:i + 1], 0.0, [k + "ss"])
                    act(junk, xt[i], AF.Square, [k + "xt"], [tag + "junk", k + "ss"], accum=ss[:, i:i + 1])
                    rstd_from_ss(rs[:, i:i + 1], ss[:, i:i + 1], D, k)
                    ts("dve", xn[i], xt[i], rs[:, i:i + 1], None, ALU.mult, None, [k + "xt", k + "rs"], [k + "xn"])
                    pt, ptk = nextpt()
                    ptv = pt.rearrange("p (k t) -> p k t", k=8)
                    for kc in range(8):
                        tr(ptv[:, kc, :], xn[i][:, kc * 128:(kc + 1) * 128], IDB, [k + "xn", "IDB"], [ptk])
                    tt("dve", tmp[i], ptv, Gv.unsqueeze(2).to_broadcast([128, 8, 128]), ALU.mult, [ptk, "G1", "G2"], [k + "tmp"])
                    tt("pool", hT[:, :, HP + t * 128:HP + (t + 1) * 128], tmp[i], Sv.unsqueeze(2).to_broadcast([128, 8, 128]), ALU.add,
                       [k + "tmp", "S1", "S2"], [hkey])
                P.barrier()
                A.reset(mk)

            def inproj_tok(hT, hkey, ntiles, col0, W, conv, evac, ring, tag):
                wr, wk, cw = ring
                dma("pool", wr[:, :, 0:W], w_in_v[:, :, col0:col0 + W], "ip_wr", [], ["ip_wr"])
                if conv:
                    c0 = col0 - X0
                    for k in range(3):
                        dma("sp", cw[:, k, 0:W], convw_d[k:k + 1, c0:c0 + W].partition_broadcast(128), "ip_cw%d" % k, [], ["ip_cw%d" % k])
                        tt("dve" if k != 1 else "pool", wk[k][:, :, 0:W], wr[:, :, 0:W], cw[:, k, 0:W].unsqueeze(1).to_broadcast([128, 8, W]), ALU.mult,
                           ["ip_wr", "ip_cw%d" % k], ["ip_wk%d" % k])
                for t in range(ntiles):
                    ps, pk = nextps()
                    if conv:
                        n = 0
                        for k in range(3):
                            for kc in range(8):
                                mm(ps[:, 0:W], hT[:, kc, HP - 1 + t * 128 + k:HP - 1 + t * 128 + k + 128], wk[k][:, kc, 0:W], n == 0, False,
                                   [hkey, "ip_wk%d" % k], [pk])
                                n += 1
                        mm(ps[:, 0:W], ones_bf[0:1, 0:128], convb_bf[0:1, c0:c0 + W], False, True, ["ones_bf", "convb_bf"], [pk])
                    else:
                        for kc in range(8):
                            mm(ps[:, 0:W], hT[:, kc, HP + t * 128:HP + (t + 1) * 128], wr[:, kc, 0:W], kc == 0, kc == 7, [hkey, "ip_wr"], [pk])
                    evac(t, ps[:, 0:W], pk)

            def softplus_dt(dt_all, n, key):
                mk = A.mark()
                ta = A.f32(n)
                stt("dve", ta, dt_all, -1.0, dt_all, ALU.mult, ALU.max, [key], [key + "ta"])
                act(ta, ta, AF.Exp, [key + "ta"], [key + "ta"], scale=-1.0)
                act(ta, ta, AF.Ln, [key + "ta"], [key + "ta"], bias=1.0)
                stt("dve", dt_all, dt_all, 0.0, ta, ALU.max, ALU.add, [key, key + "ta"], [key])
                P.barrier()
                A.reset(mk)

            def ssd_scalars(dt_all, ntiles, bufs, key):
                dtA, acs, e_all, nacs, cd, wdec = bufs
                n = ntiles * 32
                v3 = lambda a: a.rearrange("p (t c) -> p t c", c=32)
                tt("dve", v3(dtA), v3(dt_all), A_bc.unsqueeze(1).to_broadcast([128, ntiles, 32]), ALU.mult, [key, "A_bc"], [key + "dtA"])
                ps1, pk1 = nextps()
                mm(ps1[:, 0:n], U, dtA, True, True, ["U", key + "dtA"], [pk1])
                cp("dve", v3(acs)[:, :, 0:16], v3(ps1[:, 0:n])[:, :, 0:16], [pk1], [key + "acs"])
                chk("sc1")
                ps2, pk2 = nextps()
                mm(ps2[:, 0:n], L, dtA, True, True, ["L", key + "dtA"], [pk2])
                cp("dve", v3(acs)[:, :, 16:32], v3(ps2[:, 0:n])[:, :, 16:32], [pk2], [key + "acs"])
                ps3, pk3 = nextps()
                mm(ps3[:, 0:n], ONESF, dtA, True, True, ["ONESF", key + "dtA"], [pk3])
                chk("sc2")
                act(e_all, acs, AF.Exp, [key + "acs"], [key + "e"])
                ts("dve", nacs, acs, -1.0, None, ALU.mult, None, [key + "acs"], [key + "nacs"])
                act(cd, ps3[:, 0:n], AF.Exp, [pk3], [key + "cd"])
                chk("sc3")
                tt("dve", wdec, ps3[:, 0:n], nacs, ALU.add, [pk3, key + "nacs"], [key + "wdec"])
                chk("sc4")
                act(wdec, wdec, AF.Exp, [key + "wdec"], [key + "wdec"])
                chk("sc5")
                tt("dve", wdec, wdec, dt_all, ALU.mult, [key + "wdec", key], [key + "wdec"])

            def bc64(ap16, nh=16):
                return ap16.unsqueeze(2).to_broadcast([128, nh, 64])

            def hv(ap, nh=16):
                return ap.rearrange("p (h d) -> p h d", h=nh)

            def state_update(H, Hkey, xs_tile, xs_key, Btok_tile, B_key, wdec16, cd16, sc_key, xdec, tag):
                tt("dve", hv(xdec), hv(xs_tile), bc64(wdec16), ALU.mult, [xs_key, sc_key + "wdec"], [tag + "xdec"])
                tt("dve", hv(H), hv(H), bc64(cd16), ALU.mult, [Hkey, sc_key + "cd"], [Hkey])
                for g in range(2):
                    ps, pk = nextps()
                    mm(ps, Btok_tile[:, g * 128:(g + 1) * 128], xdec[:, g * 512:(g + 1) * 512], True, True, [B_key, tag + "xdec"], [pk])
                    tt("dve", H[:, g * 512:(g + 1) * 512], H[:, g * 512:(g + 1) * 512], ps, ALU.add, [Hkey, pk], [Hkey])

            for b in range(NB):
                bm = A.mark()
                xs = A.bf16(NT * D).rearrange("p (t c) -> p t c", t=NT)
                Btok = A.bf16(NT * 256).rearrange("p (t c) -> p t c", t=NT)
                BT = A.bf16(2 * SEQ).rearrange("p (g t) -> p g t", g=2)
                CT = A.bf16(2 * SEQ).rearrange("p (g t) -> p g t", g=2)
                dt_all = A.f32(NT * 32)
                scal = [A.f32(NT * 32) for _ in range(6)]
                dtA, acs, e_all, nacs, cd, wdec = scal
                v3 = lambda a: a.rearrange("p (t c) -> p t c", c=32)

                cm = A.mark()
                hcT = A.bf16(8 * (CTX + 2 * HP)).rearrange("p (k t) -> p k t", k=8)
                ring = (A.bf16(8 * 256).rearrange("p (k n) -> p k n", k=8),
                        [A.bf16(8 * 256).rearrange("p (k n) -> p k n", k=8) for _ in range(3)],
                        A.f32(3 * 256).rearrange("p (k n) -> p k n", k=3))
                norm_mod_T(lambda t: ctx_d[b, t * 128:(t + 1) * 128, :], 2, G1[:, 2, :], S1[:, 2, :], hcT, "hcT", "cn")
                chk("ctx_a")
                for gq in range(4):
                    inproj_tok(hcT, "hcT", 2, X0 + gq * 256, 256, True,
                               lambda t, ps, pk, gq=gq: act(xs[:, t, gq * 256:(gq + 1) * 256], ps, AF.Silu, [pk], ["xs"]), ring, "c")
                chk("ctx_b")
                inproj_tok(hcT, "hcT", 2, B0, 256, True, lambda t, ps, pk: act(Btok[:, t, :], ps, AF.Silu, [pk], ["Btok"]), ring, "c")
                inproj_tok(hcT, "hcT", 2, DT0, 32, False,
                           lambda t, ps, pk: tt("dve", dt_all[:, t * 32:(t + 1) * 32], ps, dtb_bc, ALU.add, [pk, "dtb_bc"], ["dt_all"]), ring, "c")
                P.barrier()
                chk("ctx_c")
                softplus_dt(dt_all[:, 0:64], 64, "dt_all")
                chk("ctx_d")
                ssd_scalars(dt_all[:, 0:64], 2, [a[:, 0:64] for a in scal], "dt_all")
                chk("ctx_e")
                xdec = A.bf16(D)
                memset("dve", H0[0], 0.0, ["H0f"])
                memset("dve", H0[1], 0.0, ["H0b"])
                for c in (0, 1):
                    state_update(H0[0], "H0f", xs[:, c, :], "xs", Btok[:, c, :], "Btok", v3(wdec)[:, c, 0:16], v3(cd)[:, c, 0:16], "dt_all", xdec, "cf")
                for c in (1, 0):
                    state_update(H0[1], "H0b", xs[:, c, :], "xs", Btok[:, c, :], "Btok", v3(wdec)[:, c, 16:32], v3(cd)[:, c, 16:32], "dt_all", xdec, "cb")
                if debug:
                    dma("sp", dbg_h0[b, 0], H0[0], "dbg0", ["H0f"], ["dbg_h0"])
                    dma("sp", dbg_h0[b, 1], H0[1], "dbg0", ["H0b"], ["dbg_h0"])
                P.barrier()
                A.reset(cm)
                chk("ctx")

                pm_ = A.mark()
                hT = A.bf16(8 * (SEQ + 2 * HP)).rearrange("p (k t) -> p k t", k=8)
                norm_mod_T(lambda t: x_d[b, t * 128:(t + 1) * 128, :], NT, G1[:, b, :], S1[:, b, :], hT, "hT", "xn")
                if debug:
                    dma("sp", dbg_hT[b], hT.rearrange("p k t -> p (k t)"), "dbg1", ["hT"], ["dbg_hT"])

                chk("p1")
                p2m = A.mark()
                ring = (A.bf16(8 * 256).rearrange("p (k n) -> p k n", k=8),
                        [A.bf16(8 * 256).rearrange("p (k n) -> p k n", k=8) for _ in range(3)],
                        A.f32(3 * 256).rearrange("p (k n) -> p k n", k=3))
                szt = [A.bf16(256) for _ in range(4)]
                szc = [0]

                def evac_z(t, ps, pk, gq):
                    i = szc[0] % 4
                    szc[0] += 1
                    act(szt[i], ps, AF.Silu, [pk], ["szt%d" % i])
                    dma("sp", sz_d[b, t * 128:(t + 1) * 128, gq * 256:(gq + 1) * 256], szt[i], "szo%d" % i, ["szt%d" % i], [uk()])

                for gq in range(4):
                    inproj_tok(hT, "hT", NT, gq * 256, 256, False, lambda t, ps, pk, gq=gq: evac_z(t, ps, pk, gq), ring, "l")
                for gq in range(4):
                    inproj_tok(hT, "hT", NT, X0 + gq * 256, 256, True,
                               lambda t, ps, pk, gq=gq: act(xs[:, t, gq * 256:(gq + 1) * 256], ps, AF.Silu, [pk], ["xs"]), ring, "l")
                inproj_tok(hT, "hT", NT, B0, 256, True, lambda t, ps, pk: act(Btok[:, t, :], ps, AF.Silu, [pk], ["Btok"]), ring, "l")
                inproj_tok(hT, "hT", NT, DT0, 32, False,
                           lambda t, ps, pk: tt("dve", dt_all[:, t * 32:(t + 1) * 32], ps, dtb_bc, ALU.add, [pk, "dtb_bc"], ["dt_all"]), ring, "l")
                wr, wk, cw = ring
                for q in range(4):
                    col0 = B0 + q * 128
                    c0 = col0 - X0
                    dst = (BT if q < 2 else CT)
                    dkey = "BT" if q < 2 else "CT"
                    g = q % 2
                    dma("pool", wr[:, :, 0:128], w_in_v[:, :, col0:col0 + 128], "ip_wr", [], ["ip_wr"])
                    for k in range(3):
                        dma("sp", cw[:, k, 0:128], convw_d[k:k + 1, c0:c0 + 128].partition_broadcast(128), "ip_cw%d" % k, [], ["ip_cw%d" % k])
                        tt("dve", wk[k][:, :, 0:128], wr[:, :, 0:128], cw[:, k, 0:128].unsqueeze(1).to_broadcast([128, 8, 128]), ALU.mult,
                           ["ip_wr", "ip_cw%d" % k], ["ip_wk%d" % k])
                    for tg in range(4):
                        ps, pk = nextps()
                        n = 0
                        for k in range(3):
                            for kc in range(8):
                                mm(ps, wk[k][:, kc, 0:128], hT[:, kc, HP - 1 + tg * 512 + k:HP - 1 + tg * 512 + k + 512], n == 0, n == 23, ["hT", "ip_wk%d" % k], [pk])
                                n += 1
                        act(dst[:, g, tg * 512:(tg + 1) * 512], ps, AF.Silu, [pk, "convbT"], [dkey], bias=convbT[:, 8 + q:9 + q])
                P.barrier()
                A.reset(p2m)
                wsc = [A.bf16(3 * 8 * 128).rearrange("p (a k n) -> p a k n", a=3, k=8) for _ in range(2)]
                c_sb = [A.f32(512) for _ in range(2)]
                vbufs = [A.bf16(SEQ + 128) for _ in range(2)]
                b_sbs = [A.bf16(SEQ) for _ in range(2)]
                t1 = A.f32(SEQ)
                ysc = [A.bf16(SEQ) for _ in range(2)]
                for s_ in range(2):
                    memset("dve", vbufs[s_][:, 0:64], 0.0, ["vbuf%d" % s_])
                    memset("dve", vbufs[s_][:, SEQ + 64:SEQ + 128], 0.0, ["vbuf%d" % s_])
                for j in range(8):
                    s = j % 2
                    vbuf, b_sb = vbufs[s], b_sbs[s]
                    for a in range(3):
                        cs = SC0 + a * D + j * 128
                        dma("pool", wsc[s][:, a, :, :], w_in_v[:, :, cs:cs + 128], "wsc%d" % s, [], ["wsc%d" % s])
                    for tg in range(4):
                        pss = []
                        for a in range(3):
                            ps, pk = nextps()
                            for kc in range(8):
                                mm(ps, wsc[s][:, a, kc, :], hT[:, kc, HP + tg * 512:HP + (tg + 1) * 512], kc == 0, kc == 7, ["hT", "wsc%d" % s], [pk])
                            pss.append((ps, pk))
                        i = tg % 2
                        cp("act", b_sb[:, tg * 512:(tg + 1) * 512], pss[0][0], [pss[0][1]], ["b_sb%d" % s])
                        cp("act", c_sb[i], pss[1][0], [pss[1][1]], ["c_sb%d" % i])
                        tt("dve", vbuf[:, 64 + tg * 512:64 + (tg + 1) * 512], pss[2][0], c_sb[i], ALU.mult, [pss[2][1], "c_sb%d" % i], ["vbuf%d" % s])
                    ts("dve", t1, vbuf[:, 0:SEQ], scwT[:, j, 0:1], None, ALU.mult, None, ["vbuf%d" % s, "scwT"], ["sc_t1"])
                    stt("dve", t1, vbuf[:, 64:64 + SEQ], scwT[:, j, 1:2], t1, ALU.mult, ALU.add, ["vbuf%d" % s, "scwT", "sc_t1"], ["sc_t1"])
                    stt("dve", t1, vbuf[:, 128:128 + SEQ], scwT[:, j, 2:3], t1, ALU.mult, ALU.add, ["vbuf%d" % s, "scwT", "sc_t1"], ["sc_t1"])
                    tt("dve", ysc[s], t1, b_sb, ALU.mult, ["sc_t1", "b_sb%d" % s], ["ysc%d" % s])
                    dma("sp", yscT_d[b, j], ysc[s], "ysco%d" % s, ["ysc%d" % s], [uk()])
                P.barrier()
                A.reset(pm_)
                if debug:
                    dma("sp", dbg_xs[b], xs.rearrange("p t c -> p (t c)"), "dbg2", ["xs"], ["dbg_xs"])
                    dma("sp", dbg_BT[b, 0], BT.rearrange("p g t -> p (g t)"), "dbg3", ["BT"], ["dbg_BT"])
                    dma("sp", dbg_BT[b, 1], CT.rearrange("p g t -> p (g t)"), "dbg3", ["CT"], ["dbg_BT"])

                chk("p2")
                softplus_dt(dt_all, NT * 32, "dt_all")
                for t0 in range(0, NT, 2):
                    ssd_scalars(dt_all[:, t0 * 32:(t0 + 2) * 32], 2, [a[:, t0 * 32:(t0 + 2) * 32] for a in scal], "dt_all")
                if debug:
                    dma("sp", dbg_dt[b], dt_all, "dbg4", ["dt_all"], ["dbg_dt"])

                if b == 0:
                    memset("dve", zt, 0.0, ["zt"])
                sm = A.mark()
                Hb_all = A.bf16(NT * D).rearrange("p (t c) -> p t c", t=NT)
                Hb = A.f32(D)
                Hf = A.f32(D)
                Hf_bf = A.bf16(D)
                xdec = A.bf16(D)
                xdt = [A.bf16(D) for _ in range(2)]
                xsD = A.bf16(D)
                cp("dve", Hb, H0[1], ["H0b"], ["Hb"])
                for c in range(NT - 1, -1, -1):
                    cp("act", Hb_all[:, c, :], Hb, ["Hb"], ["Hb_all%d" % c])
                    state_update(Hb, "Hb", xs[:, c, :], "xs", Btok[:, c, :], "Btok", v3(wdec)[:, c, 16:32], v3(cd)[:, c, 16:32], "dt_all", xdec, "bs")

                chk("bsweep")
                CBT = [A.f32(128) for _ in range(2)]
                Eb = [A.f32(128) for _ in range(6)]
                MTb = [A.bf16(128) for _ in range(6)]
                ytile = A.f32(D)
                tA = A.f32(512)
                tB = A.f32(512)
                szl = [A.bf16(D) for _ in range(2)]
                yg = A.f32(D)
                junk = A.f32(512)
                ss = A.f32(2)
                rs = A.f32(2)
                ymix = A.bf16(D)
                ymT = [A.bf16(D).rearrange("p (k t) -> p k t", k=8) for _ in range(2)]
                cp("dve", Hf, H0[0], ["H0f"], ["Hf"])
                cp("act", Hf_bf, Hf, ["Hf"], ["Hf_bf"])
                ec = [0]
                units = [(c_, g_, hh_, d_) for c_ in range(NT) for g_ in range(2) for hh_ in range(8) for d_ in range(2)]
                LEAD = 3
                cb_done = set()
                slot_of = {}
                psn[0] = 4

                def ensureCB(c_, g_):
                    if (c_, g_) in cb_done:
                        return
                    cb_done.add((c_, g_))
                    psb, pkb = nextps()
                    mm(psb[:, 0:128], BT[:, g_, c_ * 128:(c_ + 1) * 128], CT[:, g_, c_ * 128:(c_ + 1) * 128], True, True, ["BT", "CT"], [pkb])
                    cp("act", CBT[g_], psb[:, 0:128], [pkb], ["CBT%d" % g_])

                def emitA(ui):
                    c_, g_, hh_, d_ = units[ui]
                    ensureCB(c_, g_)
                    h_ = g_ * 8 + hh_
                    col = d_ * 16 + h_
                    psr, pkr = nextps()
                    mm(psr[:, 0:128], v3(dtA)[:, c_, col:col + 1].to_broadcast([128, 128]), U if d_ == 0 else L, True, False,
                       ["dt_alldtA", "U", "L"], [pkr])
                    mm(psr[:, 0:128], IDF, NEGF if d_ == 0 else NEGB, False, True, ["IDF", "NEGF", "NEGB"], [pkr])
                    ei = ec[0] % 6
                    ec[0] += 1
                    slot_of[ui] = ei
                    act(Eb[ei], psr[:, 0:128], AF.Exp, [pkr, "dt_allnacs"], ["E%d" % ei], bias=v3(nacs)[:, c_, col:col + 1])
                    tt("dve", MTb[ei], Eb[ei], CBT[g_], ALU.mult, ["E%d" % ei, "CBT%d" % g_], ["MT%d" % ei])

                def emitB(ui, psy, pky):
                    c_, g_, hh_, d_ = units[ui]
                    h_ = g_ * 8 + hh_
                    ei = slot_of[ui]
                    mm(psy[:, hh_ * 64:(hh_ + 1) * 64], MTb[ei], xdt[d_][:, h_ * 64:(h_ + 1) * 64], False, (hh_ == 7 and d_ == 1),
                       ["MT%d" % ei, "xdt%d" % d_], [pky])

                for ui in range(LEAD):
                    emitA(ui)
                xs_v = xsorted_d.rearrange("(p r) n -> p (r n)", p=128)
                ZW = 1536
                nz = (NROWS // 128) * D // ZW
                zper = -(-nz // NT)
                for c in range(NT):
                    ci = c % 2
                    if b == 0:
                        for zi in range(c * zper, min((c + 1) * zper, nz)):
                            dma("sp", xs_v[:, zi * ZW:(zi + 1) * ZW], zt[:, 0:ZW], "zfill", ["zt"],
                                (["xsorted_d"] if zi == nz - 1 else []) + ["zf%d" % (zi % 8)], background=True)
                    dma("sp", szl[ci], sz_d[b, c * 128:(c + 1) * 128, :], "szl%d" % ci, ["sz_d"], ["szl%d" % ci])
                    tt("dve", hv(xdt[0]), hv(xs[:, c, :]), bc64(v3(dt_all)[:, c, 0:16]), ALU.mult, ["xs", "dt_all"], ["xdt0"])
                    tt("pool", hv(xdt[1]), hv(xs[:, c, :]), bc64(v3(dt_all)[:, c, 16:32]), ALU.mult, ["xs", "dt_all"], ["xdt1"])
                    tt("pool", hv(xsD), hv(xs[:, c, :]), bc64(D_bc), ALU.mult, ["xs", "D_bc"], ["xsD"])
                    tt("dve", hv(xdec), hv(xs[:, c, :]), bc64(v3(wdec)[:, c, 0:16]), ALU.mult, ["xs", "dt_allwdec"], ["fsxdec"])
                    for g in range(2):
                        psy, pky = (psf[5][:, :], "ps5") if g == 0 else (psf[4][:, :], "ps4")
                        mm(psy, IDB, xsD[:, g * 512:(g + 1) * 512], True, False, ["IDB", "xsD"], [pky])
                        for hh in range(8):
                            for d in range(2):
                                ui = ((c * 2 + g) * 8 + hh) * 2 + d
                                emitB(ui, psy, pky)
                                if ui + LEAD < len(units):
                                    emitA(ui + LEAD)
                        pof, pkf = nextps()
                        mm(pof, CT[:, g, c * 128:(c + 1) * 128], Hf_bf[:, g * 512:(g + 1) * 512], True, True, ["CT", "Hf_bf"], [pkf])
                        pob, pkb2 = nextps()
                        mm(pob, CT[:, g, c * 128:(c + 1) * 128], Hb_all[:, c, g * 512:(g + 1) * 512], True, True, ["CT", "Hb_all%d" % c], [pkb2])
                        tt("dve", hv(tA, 8), hv(pof, 8), bc64(v3(e_all)[:, c, g * 8:(g + 1) * 8], 8), ALU.mult, [pkf, "dt_alle"], ["tA"])
                        tt("dve", hv(tB, 8), hv(pob, 8), bc64(v3(e_all)[:, c, 16 + g * 8:16 + (g + 1) * 8], 8), ALU.mult, [pkb2, "dt_alle"], ["tB"])
                        tt("pool", tA, tA, tB, ALU.add, ["tA", "tB"], ["tA"])
                        tt("dve", ytile[:, g * 512:(g + 1) * 512], psy, tA, ALU.add, [pky, "tA"], ["ytile"])
                    tt("dve", hv(Hf), hv(Hf), bc64(v3(cd)[:, c, 0:16]), ALU.mult, ["Hf", "dt_allcd"], ["Hf"])
                    for g in range(2):
                        ps, pk = nextps()
                        mm(ps, Btok[:, c, g * 128:(g + 1) * 128], xdec[:, g * 512:(g + 1) * 512], True, True, ["Btok", "fsxdec"], [pk])
                        tt("dve", Hf[:, g * 512:(g + 1) * 512], Hf[:, g * 512:(g + 1) * 512], ps, ALU.add, ["Hf", pk], ["Hf"])
                    cp("act", Hf_bf, Hf, ["Hf"], ["Hf_bf"])
                    tt("dve", yg, ytile, szl[ci], ALU.mult, ["ytile", "szl%d" % ci], ["yg"])
                    for g in range(2):
                        memset("dve", ss[:, g:g + 1], 0.0, ["gss"])
                        act(junk, yg[:, g * 512:(g + 1) * 512], AF.Square, ["yg"], ["gjunk", "gss"], accum=ss[:, g:g + 1])
                    rstd_from_ss(rs, ss, 512, "g")
                    for g in range(2):
                        stt("dve", ymix[:, g * 512:(g + 1) * 512], yg[:, g * 512:(g + 1) * 512], rs[:, g:g + 1], gssd_bc[:, g * 512:(g + 1) * 512],
                            ALU.mult, ALU.mult, ["yg", "grs", "gssd_bc"], ["ymix"])
                    pt, ptk = nextpt()
                    ptv = pt.rearrange("p (k t) -> p k t", k=8)
                    for kc in range(8):
                        tr(ptv[:, kc, :], ymix[:, kc * 128:(kc + 1) * 128], IDB, ["ymix", "IDB"], [ptk])
                    cp("act", ymT[ci], ptv, [ptk], ["ymT%d" % ci])
                    dma("sp", yssdT_d[b].rearrange("k p t -> p k t")[:, :, c * 128:(c + 1) * 128], ymT[ci], "ymo%d" % ci, ["ymT%d" % ci], [uk()])
                P.barrier()
                A.reset(bm)

                psn[0] = 5
                chk("sweep")
                w_out_bf = A.bf16(16 * D).rearrange("p (k n) -> p k n", k=16)
                ymg = [A.bf16(16 * 512).rearrange("p (k t) -> p k t", k=16) for _ in range(2)]
                xt = [A.f32(D) for _ in range(2)]
                x1 = [A.f32(D) for _ in range(2)]
                tPs = [A.f32(D) for _ in range(2)]
                junk = A.f32(D)
                ss = A.f32(2)
                rs = A.f32(2)
                xn2s = [A.bf16(D) for _ in range(2)]
                tmp = A.f32(D).rearrange("p (k t) -> p k t", k=8)
                h2f = A.f32(D).rearrange("p (k t) -> p k t", k=8)
                h2b = [A.bf16(D).rearrange("p (k t) -> p k t", k=8) for _ in range(2)]
                h2tok = [A.bf16(D) for _ in range(2)]
                lg = A.f32(NE)
                mx8 = A.f32(8)
                nmx = A.f32(1)
                msk = A.f32(NE)
                ex = A.f32(NE)
                sm_ = A.f32(1)
                for hf in range(2):
                    dma("pool", w_out_bf[:, hf * 8:(hf + 1) * 8, :], w_out_v[:, hf * 8:(hf + 1) * 8, :], "wout", [], ["w_out_bf"])
                def p4_s1(t):
                    tg, tl = t // 4, t % 4
                    gi = tg % 2
                    i = t % 2
                    if tl == 0:
                        dma("sp", ymg[gi][:, 0:8, :], yssdT_d[b].rearrange("k p t -> p k t")[:, :, tg * 512:(tg + 1) * 512], "ymg%d" % gi, ["yssdT_d"], ["ymg%d" % gi])
                        dma("sp", ymg[gi][:, 8:16, :], yscT_d[b].rearrange("k p t -> p k t")[:, :, tg * 512:(tg + 1) * 512], "ymgb%d" % gi, ["yscT_d"], ["ymgb%d" % gi])
                    dma("sp", xt[i], x_d[b, t * 128:(t + 1) * 128, :], "p4x%d" % i, [], ["p4xt%d" % i])
                    p4ps[t] = []
                    for hf in range(2):
                        ps, pk = nextps()
                        for kc in range(16):
                            mm(ps, ymg[gi][:, kc, tl * 128:(tl + 1) * 128], w_out_bf[:, kc, hf * 512:(hf + 1) * 512], kc == 0, kc == 15,
                               ["ymg%d" % gi, "ymgb%d" % gi, "w_out_bf"], [pk])
                        p4ps[t].append((ps, pk))

                def p4_s1b(t):
                    i = t % 2
                    for hf in range(2):
                        ps, pk = p4ps[t][hf]
                        tt("dve", tPs[i][:, hf * 512:(hf + 1) * 512], ps, g1_bc[b][:, hf * 512:(hf + 1) * 512], ALU.mult, [pk, "g1_bc%d" % b], ["tP%d" % i])
                    tt("dve", x1[i], xt[i], tPs[i], ALU.add, ["p4xt%d" % i, "tP%d" % i], ["x1_%d" % i])
                    dma("sp", x1_d[b, t * 128:(t + 1) * 128, :], x1[i], "x1o%d" % i, ["x1_%d" % i], [uk()])
                    memset("dve", ss[:, i:i + 1], 0.0, ["p4%dss" % i])
                    act(junk, x1[i], AF.Square, ["x1_%d" % i], ["p4junk", "p4%dss" % i], accum=ss[:, i:i + 1])
                    rstd_from_ss(rs[:, i:i + 1], ss[:, i:i + 1], D, "p4%d" % i)
                    ts("dve", xn2s[i], x1[i], rs[:, i:i + 1], None, ALU.mult, None, ["x1_%d" % i, "p4%drs" % i], ["xn2_%d" % i])

                def p4_s2(t):
                    i = t % 2
                    pt, ptk = nextpt()
                    ptv = pt.rearrange("p (k t) -> p k t", k=8)
                    for kc in range(8):
                        tr(ptv[:, kc, :], xn2s[i][:, kc * 128:(kc + 1) * 128], IDB, ["xn2_%d" % i, "IDB"], [ptk])
                    tt("dve", tmp, ptv, G2[:, b, :].unsqueeze(2).to_broadcast([128, 8, 128]), ALU.mult, [ptk, "G2"], ["p4tmp"])
                    tt("pool", h2f, tmp, S2[:, b, :].unsqueeze(2).to_broadcast([128, 8, 128]), ALU.add, ["p4tmp", "S2"], ["h2f"])
                    cp("act", h2b[i], h2f, ["h2f"], ["h2b%d" % i])
                    pt2, ptk2 = nextpt()
                    ptv2 = pt2.rearrange("p (k t) -> p k t", k=8)
                    for kc in range(8):
                        tr(ptv2[:, kc, :], h2b[i][:, kc, :], IDB, ["h2b%d" % i, "IDB"], [ptk2])
                    cp("act", h2tok[i], pt2, [ptk2], ["h2tok%d" % i])
                    dma("sp", h2tok_d[b * SEQ + t * 128:b * SEQ + (t + 1) * 128, :], h2tok[i], "h2o%d" % i, ["h2tok%d" % i], [uk()])
                    ps, pk = nextps()
                    for kc in range(8):
                        mm(ps[:, 0:NE], h2f[:, kc, :], wr_f[:, kc, :], kc == 0, kc == 7, ["h2f", "wr_f"], [pk])
                    tt("dve", lg, ps[:, 0:NE], br_bc, ALU.add, [pk, "br_bc"], ["lg"])
                    P.op("dve", lambda e, o=mx8, i_=lg: e.max(out=o, in_=i_), ["lg"], ["mx8"])
                    ts("dve", msk, lg, mx8[:, 3:4], None, ALU.is_ge, None, ["lg", "mx8"], ["msk"])
                    ts("dve", nmx, mx8[:, 0:1], -1.0, None, ALU.mult, None, ["mx8"], ["nmx"])
                    act(ex, lg, AF.Exp, ["lg", "nmx"], ["ex"], bias=nmx)
                    tt("dve", ex, ex, msk, ALU.mult, ["ex", "msk"], ["ex"])
                    P.op("dve", lambda e, o=sm_, i_=ex: e.reduce_sum(out=o, in_=i_, axis=AX.X), ["ex"], ["sm"])
                    recip(sm_, sm_, ["sm"], ["sm"])
                    ts("dve", gates[b][:, t, :], ex, sm_, None, ALU.mult, None, ["ex", "sm"], ["gates%d" % b])

                p4ps = {}
                p4_s1(0)
                p4_s1b(0)
                for t in range(NT):
                    if t + 1 < NT:
                        p4_s1(t + 1)
                    p4_s2(t)
                    if t + 1 < NT:
                        p4_s1b(t + 1)
                if debug:
                    dma("sp", dbg_gates[b], gates[b].rearrange("p t e -> p (t e)"), "dbg5", ["gates%d" % b], ["dbg_gates"])
                P.barrier()
                A.reset(bm)

            A.reset(perm_mark)
            if stop_after == "mixer":
                return
            BS = MBS
            NBLK = MNBLK
            NSUB = BS // 128
            NTT = NB * NT
            rm = A.mark()
            dsel_i = A.f32(NTT * 4).bitcast(I32).rearrange("p (t k) -> p t k", k=4)
            gk = A.f32(NTT * 4).rearrange("p (t k) -> p t k", k=4)
            widx_i = A.f32(NBLK * 8).bitcast(I32).rearrange("p (b k) -> p b k", k=8)
            wgidx_i = A.f32(NBLK * 8).bitcast(I32).rearrange("p (b k) -> p b k", k=8)
            bidx_i = A.f32(NBLK).bitcast(I32)
            eb_i = A.f32(NBLK).bitcast(I32)
            idx_mark = A.mark()
            SU_bf = A.bf16(128)
            pk_f = A.f32(8)
            Mf = A.f32(NTT * NE)
            M_bf = A.bf16(NTT * NE)
            rk = A.f32(NTT * NE)
            cntb = A.f32(NTT * NE)
            offs = A.f32(NTT * NE)
            dest1 = A.f32(NTT * NE)
            count = A.f32(NE)
            nblk = A.f32(NE)
            pstart = A.f32(NE)
            pend = A.f32(NE)
            mx8 = A.f32(8)
            tmpe = A.f32(NE)
            dsel_f = A.f32(NTT * 4).rearrange("p (t k) -> p t k", k=4)
            ebf = A.f32(NBLK)
            empt = A.f32(NBLK)
            tmpb = A.f32(NBLK)
            widx_f = A.f32(NBLK * 8).rearrange("p (b k) -> p b k", k=8)
            v3e = lambda a: a.rearrange("p (t e) -> p t e", e=NE)
            gall = gates_all.rearrange("p t e -> p (t e)")
            dma("pool", SU_bf, SU_d, "r_su", [], ["SU_bf"])
            dma("sp", pk_f, PK_d, "r_pk", [], ["pk_f"])
            ts("dve", Mf, gall, 0.0, None, ALU.is_gt, None, ["gates0", "gates1"], ["Mf"])
            cp("dve", M_bf, Mf, ["Mf"], ["M_bf"])
            for hf in range(2):
                ps, pk = nextps()
                mm(ps, SU_bf, M_bf[:, hf * 512:(hf + 1) * 512], True, True, ["SU_bf", "M_bf"], [pk])
                cp("dve", rk[:, hf * 512:(hf + 1) * 512], ps, [pk], ["rk"])
                ps, pk = nextps()
                mm(ps, ones_bf, M_bf[:, hf * 512:(hf + 1) * 512], True, True, ["ones_bf", "M_bf"], [pk])
                cp("dve", cntb[:, hf * 512:(hf + 1) * 512], ps, [pk], ["cntb"])
            memset("dve", offs[:, 0:NE], 0.0, ["offs"])
            for T in range(1, NTT):
                tt("dve", offs[:, T * NE:(T + 1) * NE], offs[:, (T - 1) * NE:T * NE], cntb[:, (T - 1) * NE:T * NE], ALU.add, ["offs", "cntb"], ["offs"])
            tt("dve", count, offs[:, (NTT - 1) * NE:NTT * NE], cntb[:, (NTT - 1) * NE:NTT * NE], ALU.add, ["offs", "cntb"], ["count"])
            memset("dve", nblk, 0.0, ["nblk"])
            for k in range(-(-(NB * SEQ) // BS)):
                stt("dve", nblk, count, float(k * BS), nblk, ALU.is_gt, ALU.add, ["count", "nblk"], ["nblk"])
            ts("dve", nblk, nblk, float(BS), None, ALU.mult, None, ["nblk"], ["nblk"])
            memset("dve", pstart[:, 0:1], 0.0, ["pstart"])
            for e in range(1, NE):
                tt("dve", pstart[:, e:e + 1], pstart[:, e - 1:e], nblk[:, e - 1:e], ALU.add, ["pstart", "nblk"], ["pstart"])
            tt("dve", pend, pstart, nblk, ALU.add, ["pstart", "nblk"], ["pend"])
            tt("dve", v3e(offs), v3e(offs), pstart.unsqueeze(1).to_broadcast([128, NTT, NE]), ALU.add, ["offs", "pstart"], ["offs"])
            tt("dve", dest1, rk, offs, ALU.add, ["rk", "offs"], ["dest1"])
            stt("dve", dest1, dest1, 1.0, Mf, ALU.add, ALU.mult, ["dest1", "Mf"], ["dest1"])
            for T in range(NTT):
                P.op("dve", lambda e, o=mx8, i_=v3e(dest1)[:, T, :]: e.max(out=o, in_=i_), ["dest1"], ["rmx8"])
                ts("dve", dsel_f[:, T, :], mx8[:, 0:4], -1.0, None, ALU.add, None, ["rmx8"], ["dsel_f"])
                for k in range(4):
                    stt("dve", tmpe, v3e(dest1)[:, T, :], mx8[:, k:k + 1], v3e(gall)[:, T, :], ALU.is_equal, ALU.mult, ["dest1", "rmx8", "gates0", "gates1"], ["tmpe"])
                    P.op("dve", lambda e, o=gk[:, T, k:k + 1], i_=tmpe: e.reduce_sum(out=o, in_=i_, axis=AX.X), ["tmpe"], ["gk"])
            cp("dve", dsel_i.rearrange("p t k -> p (t k)"), dsel_f.rearrange("p t k -> p (t k)"), ["dsel_f"], ["dsel_i"])
            for blk in range(NBLK):
                ts("dve", tmpe, pend, float(blk * BS), None, ALU.is_le, None, ["pend"], ["tmpe"])
                P.op("dve", lambda e, o=ebf[:, blk:blk + 1], i_=tmpe: e.reduce_sum(out=o, in_=i_, axis=AX.X), ["tmpe"], ["ebf"])
                cp("dve", empt[:, blk:blk + 1], tmpe[:, NE - 1:NE], ["tmpe"], ["empt"])
            ts("dve", ebf, ebf, float(NE - 1), None, ALU.min, None, ["ebf"], ["ebf"])
            ts("dve", empt, empt, 1.0e6, None, ALU.mult, None, ["empt"], ["empt"])
            cp("dve", eb_i, ebf, ["ebf"], ["eb_i"])
            ts("dve", tmpb, ebf, 128.0, None, ALU.mult, None, ["ebf", "eb_i"], ["tmpb"])
            ts("dve", tmpb, tmpb, pk_f[:, 0:1], None, ALU.add, None, ["tmpb", "pk_f"], ["tmpb"])
            cp("dve", bidx_i, tmpb, ["tmpb"], ["bidx_i"])
            ts("dve", tmpb, ebf, 1024.0, None, ALU.mult, None, ["ebf", "bidx_i"], ["tmpb"])
            tt("dve", widx_f, tmpb.unsqueeze(2).to_broadcast([128, NBLK, 8]), pk_f.unsqueeze(1).to_broadcast([128, NBLK, 8]), ALU.add, ["tmpb", "pk_f"], ["widx_f"])
            cp("dve", widx_i.rearrange("p b k -> p (b k)"), widx_f.rearrange("p b k -> p (b k)"), ["widx_f"], ["widx_i"])
            tt("dve", widx_f, widx_f, empt.unsqueeze(2).to_broadcast([128, NBLK, 8]), ALU.add, ["widx_f", "empt", "widx_i"], ["widx_f"])
            cp("dve", wgidx_i.rearrange("p b k -> p (b k)"), widx_f.rearrange("p b k -> p (b k)"), ["widx_f"], ["wgidx_i"])
            if debug:
                dma("sp", dbg_route[:, 0:NTT * 4], dsel_f.rearrange("p t k -> p (t k)"), "dbgr", ["dsel_f"], ["dbg_route"])
                dma("sp", dbg_route[:, NTT * 4:NTT * 8], gk.rearrange("p t k -> p (t k)"), "dbgr", ["gk"], ["dbg_route"])
                dma("sp", dbg_route[:, NTT * 8:NTT * 8 + NBLK], ebf, "dbgr", ["ebf"], ["dbg_route"])
            P.barrier()
            A.reset(idx_mark)
            chk("route")
            dm = A.mark()
            hx_ = [A.bf16(D) for _ in range(4)]
            for T in range(NTT):
                i = T % 4
                dma("sp", hx_[i], h2tok_d[T * 128:(T + 1) * 128, :], "dl%d" % i, ["h2tok_d"], ["hx%d" % i])
                for k in range(4):
                    P.dma("pool", lambda e, o=xsorted_d[:, :], ix=dsel_i[:, T, k:k + 1], src=hx_[i]: e.indirect_dma_start(
                        out=o, out_offset=bass.IndirectOffsetOnAxis(ap=ix, axis=0), in_=src, in_offset=None),
                        "ds%d" % i, ["hx%d" % i, "dsel_i", "xsorted_d"], [uk()])
            P.barrier()
            A.reset(dm)
            chk("dispatch")
            em = A.mark()
            Wgu = [A.bf16(8 * 2 * D).rearrange("p (k n) -> p k n", k=8) for _ in range(2)]
            Wd = [A.bf16(8 * D).rearrange("p (k n) -> p k n", k=8) for _ in range(2)]
            bgu = [A.f32(16) for _ in range(2)]
            bdr = [A.bf16(D) for _ in range(2)]
            xblk = [A.bf16(NSUB * D).rearrange("p (s n) -> p s n", s=NSUB) for _ in range(2)]
            xT = [A.bf16(8 * BS).rearrange("p (k t) -> p k t", k=8) for _ in range(2)]
            actT = [A.bf16(8 * BS).rearrange("p (k t) -> p k t", k=8) for _ in range(2)]
            yt = [A.f32(D) for _ in range(2)]
            glu = [A.f32(512) for _ in range(2)]
            sig = [A.f32(512) for _ in range(2)]
            l1 = [A.f32(512) for _ in range(2)]
            tq = [A.f32(512) for _ in range(2)]
            wgu_rows = w_gu_d.rearrange("e k n -> (e k) n")
            bregs = {}

            def breg(e, val):
                if val not in bregs:
                    r = e.alloc_register("bnd%d" % val)
                    e.reg_mov(r, val)
                    bregs[val] = r
                return bregs[val]
            wd_rows = w_down_d.rearrange("e k n -> (e k) n")
            uc = 0
            yc = 0
            for blk in range(NBLK):
                s = blk % 2
                for kc in range(8):
                    P.dma("pool", lambda e, o=Wgu[s][:, kc, :], ix=wgidx_i[:, blk, kc:kc + 1]: e.indirect_dma_start(
                        out=o, out_offset=None, in_=wgu_rows[:, :], in_offset=bass.IndirectOffsetOnAxis(ap=ix, axis=0),
                        bounds_check=breg(e, NE * D - 1), oob_is_err=False),
                        "Wgu%d" % s, ["wgidx_i"], ["Wgu%d_%d" % (s, kc)])
                P.dma("pool", lambda e, o=bgu[s], ix=bidx_i[:, blk:blk + 1]: e.indirect_dma_start(
                    out=o, out_offset=None, in_=bguFM_d[:, :], in_offset=bass.IndirectOffsetOnAxis(ap=ix, axis=0)),
                    "bgu%d" % s, ["bidx_i"], ["bgu%d" % s])
                ts("dve", bgu[s][:, 8:16], bgu[s][:, 8:16], 1.0, None, ALU.add, None, ["bgu%d" % s], ["bgu%d" % s])
                P.dma("pool", lambda e, o=bdr[s][0:2, :], ix=eb_i[0:2, blk:blk + 1]: e.indirect_dma_start(
                    out=o, out_offset=None, in_=bd_row_d[:, :], in_offset=bass.IndirectOffsetOnAxis(ap=ix, axis=0)),
                    "bdr%d" % s, ["eb_i"], ["bdr%d" % s])
                for kc in range(8):
                    P.dma("pool", lambda e, o=Wd[s][:, kc, :], ix=wgidx_i[:, blk, kc:kc + 1]: e.indirect_dma_start(
                        out=o, out_offset=None, in_=wd_rows[:, :], in_offset=bass.IndirectOffsetOnAxis(ap=ix, axis=0),
                        bounds_check=breg(e, NE * D - 1), oob_is_err=False),
                        "Wd%d" % s, ["wgidx_i"], ["Wd%d_%d" % (s, kc)])
                if blk == 0:
                    dma("sp", xblk[0], xsorted_d[0:BS, :].rearrange("(s p) n -> p s n", p=128), "xb0", ["xsorted_d"], ["xblk0"])
                for sub in range(NSUB):
                    pt, ptk = nextpt()
                    ptv = pt.rearrange("p (k t) -> p k t", k=8)
                    for kc in range(8):
                        tr(ptv[:, kc, :], xblk[s][:, sub, kc * 128:(kc + 1) * 128], IDB, ["xblk%d" % s, "IDB"], [ptk])
                    cp("act" if sub % 2 == 0 else "dve", xT[s][:, :, sub * 128:(sub + 1) * 128], ptv, [ptk], ["xT%d" % s])
                if blk + 1 < NBLK:
                    dma("sp", xblk[1 - s], xsorted_d[(blk + 1) * BS:(blk + 2) * BS, :].rearrange("(s p) n -> p s n", p=128), "xb%d" % (1 - s),
                        ["xsorted_d"], ["xblk%d" % (1 - s)])
                for j in range(8):
                    i = uc % 2
                    uc += 1
                    psg, pkg = nextps()
                    for kc in range(8):
                        mm(psg[:, 0:BS], Wgu[s][:, kc, j * 128:(j + 1) * 128], xT[s][:, kc, :], kc == 0, kc == 7, ["Wgu%d_%d" % (s, kc), "xT%d" % s], [pkg])
                    psl, pkl = nextps()
                    for kc in range(8):
                        mm(psl[:, 0:BS], Wgu[s][:, kc, D + j * 128:D + (j + 1) * 128], xT[s][:, kc, :], kc == 0, kc == 7, ["Wgu%d_%d" % (s, kc), "xT%d" % s], [pkl])
                    ts("dve", glu[i][:, 0:BS], psg[:, 0:BS], bgu[s][:, j:j + 1], 7.0, ALU.add, ALU.min, [pkg, "bgu%d" % s], ["glu%d" % i])
                    act(sig[i][:, 0:BS], glu[i][:, 0:BS], AF.Sigmoid, ["glu%d" % i], ["sig%d" % i], scale=1.702)
                    ts("dve", l1[i][:, 0:BS], psl[:, 0:BS], bgu[s][:, 8 + j:9 + j], -6.0, ALU.add, ALU.max, [pkl, "bgu%d" % s], ["l1%d" % i])
                    tt("dve", tq[i][:, 0:BS], glu[i][:, 0:BS], sig[i][:, 0:BS], ALU.mult, ["glu%d" % i, "sig%d" % i], ["tq%d" % i])
                    stt("dve", actT[s][:, j, :], l1[i][:, 0:BS], 8.0, tq[i][:, 0:BS], ALU.min, ALU.mult, ["l1%d" % i, "tq%d" % i], ["actT%d" % s])
                for sub in range(NSUB):
                    yi = yc % 2
                    yc += 1
                    for hf in range(2):
                        ps, pk = nextps()
                        mm(ps, ones_bf[0:1, 0:128], bdr[s][0:1, hf * 512:(hf + 1) * 512], True, False, ["ones_bf", "bdr%d" % s], [pk])
                        for j in range(8):
                            mm(ps, actT[s][:, j, sub * 128:(sub + 1) * 128], Wd[s][:, j, hf * 512:(hf + 1) * 512], False, j == 7, ["actT%d" % s, "Wd%d_%d" % (s, j)], [pk])
                        cp("act", yt[yi][:, hf * 512:(hf + 1) * 512], ps, [pk], ["yt%d" % yi])
                    r0 = blk * BS + sub * 128
                    dma("sp", ysorted_d[r0:r0 + 128, :], yt[yi], "yo%d" % yi, ["yt%d" % yi], [uk()])
            P.barrier()
            A.reset(em)
            chk("experts")
            yk = [[A.f32(D) for _ in range(4)] for _ in range(3)]
            acc = [A.f32(D) for _ in range(3)]
            xt = [A.f32(D) for _ in range(3)]
            junk = A.f32(D)
            ss = A.f32(2)
            rs = A.f32(2)
            for T in range(NTT):
                b, t = T // NT, T % NT
                i = T % 3
                for k in range(4):
                    P.dma("pool", lambda e, o=yk[i][k], ix=dsel_i[:, T, k:k + 1]: e.indirect_dma_start(
                        out=o, out_offset=None, in_=ysorted_d[:, :], in_offset=bass.IndirectOffsetOnAxis(ap=ix, axis=0)),
                        "yk%d_%d" % (i, k), ["dsel_i", "ysorted_d"], ["yk%d_%d" % (i, k)])
                dma("sp", xt[i], x1_d[b, t * 128:(t + 1) * 128, :], "fx%d" % i, ["x1_d"], ["fxt%d" % i])
                ts("dve", acc[i], yk[i][0], gk[:, T, 0:1], None, ALU.mult, None, ["yk%d_0" % i, "gk"], ["acc%d" % i])
                for k in range(1, 4):
                    stt("dve", acc[i], yk[i][k], gk[:, T, k:k + 1], acc[i], ALU.mult, ALU.add, ["yk%d_%d" % (i, k), "gk", "acc%d" % i], ["acc%d" % i])
                tt("dve", acc[i], acc[i], g2_bc[b], ALU.mult, ["acc%d" % i, "g2_bc%d" % b], ["acc%d" % i])
                tt("dve", acc[i], acc[i], xt[i], ALU.add, ["acc%d" % i, "fxt%d" % i], ["acc%d" % i])
                memset("dve", ss[:, 0:1], 0.0, ["fss"])
                act(junk, acc[i], AF.Square, ["acc%d" % i], ["fjunk", "fss"], accum=ss[:, 0:1])
                rstd_from_ss(rs[:, 0:1], ss[:, 0:1], D, "f")
                stt("dve", acc[i], acc[i], rs[:, 0:1], fg_bc, ALU.mult, ALU.mult, ["acc%d" % i, "frs", "fg_bc"], ["acc%d" % i])
                dma("sp", out_d[b, t * 128:(t + 1) * 128, :], acc[i], "fo%d" % i, ["acc%d" % i], [uk()], output=True)
            P.barrier()
        try:
            _body()
        except _Stop:
            pass
        P.finalize_and_emit(st)
        build.stats = P.stats
    return nc


def _consts():
    k = np.arange(128)[:, None]
    i = np.arange(128)[None, :]
    U = (k <= i).astype(np.float32)
    L = (k >= i).astype(np.float32)
    NEGF = np.where(i < k, -30000.0, 0.0).astype(np.float32)
    NEGB = np.where(i > k, -30000.0, 0.0).astype(np.float32)
    IDF = np.eye(128, dtype=np.float32)
    SU = (k < i).astype(np.float32)
    PK = (np.arange(128)[:, None] + 128 * np.arange(8)[None, :]).astype(np.float32)
    return dict(cU=U, cL=L, cNEGF=NEGF, cNEGB=NEGB, cIDF=IDF, cSU=SU, cPK=PK)


def fmaj(v, nch):
    return np.ascontiguousarray(np.asarray(v, np.float32).reshape(nch, 128).T)


def make_in_maps(inputs, cores):
    f = lambda a: np.ascontiguousarray(np.asarray(a, dtype=np.float32))
    x, c, ctx, c_ctx = f(inputs["x"]), f(inputs["c"]), f(inputs["ctx"]), f(inputs["c_ctx"])
    shared = dict(
        w_mod=f(inputs["w_mod"][0]),
        b_modT=fmaj(inputs["b_mod"][0], 48),
        b_mod_row=f(inputs["b_mod"][0]).reshape(1, -1),
        n1gT=fmaj(inputs["norm1_g"][0], 8),
        n2gT=fmaj(inputs["norm2_g"][0], 8),
        w_in=f(inputs["w_in"][0]),
        convw=f(inputs["ssd_conv_w"][0]),
        convb_row=f(inputs["ssd_conv_b"][0]).reshape(1, -1),
        convbT=fmaj(inputs["ssd_conv_b"][0], 12),
        dtb_row=f(inputs["ssd_dt_bias"][0]).reshape(1, 32),
        alog_row=f(inputs["ssd_a_log"][0]).reshape(1, 32),
        d_row=f(inputs["ssd_d"][0]).reshape(1, 16),
        gssd_row=f(inputs["ssd_norm_g"][0]).reshape(1, -1),
        scwT=np.ascontiguousarray(f(inputs["sc_conv_w"][0]).reshape(3, 8, 128).transpose(2, 1, 0)),
        w_out=f(inputs["w_out"][0]),
        w_router=f(inputs["w_router"][0]),
        br_row=f(inputs["b_router"][0]).reshape(1, -1),
        w_gu=f(inputs["w_gate_up"][0]),
        bguT=np.ascontiguousarray(f(inputs["b_gate_up"][0]).reshape(NE, 16, 128).transpose(2, 0, 1)),
        w_down=f(inputs["w_down"][0]),
        bguFM=np.ascontiguousarray(f(inputs["b_gate_up"][0]).reshape(NE, 16, 128).transpose(0, 2, 1).reshape(NE * 128, 16)),
        bd_row=f(inputs["b_down"][0]),
        fg_row=f(inputs["final_g"]).reshape(1, -1),
    )
    shared.update(_consts())
    maps = []
    for ci in cores:
        vecs = np.stack([c[2 * ci], c[2 * ci + 1], c_ctx], axis=1)
        cT = np.ascontiguousarray(vecs.reshape(8, 128, 3).transpose(1, 0, 2))
        m = dict(shared)
        m["x"] = np.ascontiguousarray(x[2 * ci:2 * ci + 2])
        m["ctx"] = np.ascontiguousarray(ctx[2 * ci:2 * ci + 2])
        m["cT"] = cT
        maps.append(m)
    return maps


def kernel(**inputs):
    nc = build(debug=False)
    maps = make_in_maps(inputs, list(range(8)))
    res = run_bass_kernel_spmd(nc, maps, core_ids=list(range(8)))
    return np.concatenate([np.asarray(r["out"], dtype=np.float32) for r in res.results], axis=0)
```
